# Optimizing a Trainium2 kernel written in Bass

```python
import math
import jax, jax.numpy as jnp
from jax import lax
import numpy as np

D_MODEL = 2048
BATCH = 4
SEQ = 2048
DEPTH = 1

D_MIX = D_MODEL
D_HY = D_MIX // 2
D_ML = D_MIX - D_HY
HY_GROUPS = 8
HY_ORDER = 2
HY_SHORT = 3
HY_EMB = 33
HY_FILT_HID = 64
HY_DECAY_TARGET = 1e-2
HY_FAST_PCT = 0.3
HY_SLOW_PCT = 1.5
ML_HEADS = 8
ML_HEAD_DIM = D_ML // ML_HEADS
ML_SHORT = 3
ML_CHUNK = 128
N_GATE_COLS = 4 * ML_HEADS
D_IN = 3 * D_HY + 4 * D_ML + N_GATE_COLS
N_EXPERTS = 32
TOP_K = 4
D_FF = D_MODEL
SWIGLU_LIMIT = 7.0
SWIGLU_ALPHA = 1.702
MOE_BLOCK = 128
LN_EPS = 1e-5
DN_ALPHA = (2 * DEPTH) ** 0.25
DN_BETA = (8 * DEPTH) ** -0.25

kernel_name = 'hyena_mlstm_parallel_moe_deepnorm'

F32 = jnp.float32


def layer_norm(u, g, b):
    uf = u.astype(F32)
    mu = uf.mean(-1, keepdims=True)
    var = jnp.square(uf - mu).mean(-1, keepdims=True)
    return ((uf - mu) * lax.rsqrt(var + LN_EPS)).astype(u.dtype) * g + b


def group_norm(u, g, groups):
    shp = u.shape
    uf = u.astype(F32).reshape(*shp[:-1], groups, shp[-1] // groups)
    mu = uf.mean(-1, keepdims=True)
    var = jnp.square(uf - mu).mean(-1, keepdims=True)
    y = ((uf - mu) * lax.rsqrt(var + LN_EPS)).reshape(shp)
    return y.astype(u.dtype) * g


def centred_conv(u, w, b):
    width = w.shape[0]
    p = width // 2
    L = u.shape[1]
    up = jnp.pad(u, ((0, 0), (p, p), (0, 0)))
    return sum(w[j] * up[:, j:j + L] for j in range(width)) + b


def hyena_filter_spectrum(w1, b1, w2, b2, w3, freq, L):
    t = jnp.linspace(0.0, 1.0, L, dtype=F32)[:, None]
    bands = (HY_EMB - 1) // 2
    fb = jnp.linspace(1e-4, bands - 1, bands, dtype=F32)[None]
    w = 2.0 * math.pi * jnp.arange(L, dtype=F32)[:, None] / L
    z = jnp.concatenate([t, jnp.cos(fb * w), -jnp.sin(fb * w)], -1)
    h = jnp.sin(freq[0].astype(F32) * (z @ w1.astype(F32) + b1.astype(F32)))
    h = jnp.sin(freq[1].astype(F32) * (h @ w2.astype(F32) + b2.astype(F32)))
    h = (h @ w3.astype(F32)).reshape(L, HY_ORDER, 2, D_HY)
    deltas = jnp.abs(jnp.linspace(math.log(HY_DECAY_TARGET) / HY_SLOW_PCT,
                                  math.log(HY_DECAY_TARGET) / HY_FAST_PCT, D_HY, dtype=F32))
    h = h * jnp.exp(-t[:, :, None, None] * deltas)
    fwd, bwd = h[:, :, 0], h[:, :, 1]
    k = jnp.concatenate([fwd, jnp.zeros_like(fwd[:1]), jnp.flip(bwd[1:], 0)], 0)
    k = k / jnp.sum(jnp.abs(k), axis=0, keepdims=True)
    return jnp.fft.rfft(k, axis=0)


def long_conv(z, kf):
    L = z.shape[1]
    zf = jnp.fft.rfft(z, n=2 * L, axis=1)
    return jnp.fft.irfft(zf * kf[None], n=2 * L, axis=1)[:, :L]


def mlstm_chunkwise(q, k, v, ig, fg):
    B, H, L, d = q.shape
    nc = L // ML_CHUNK
    lf = jax.nn.log_sigmoid(fg)

    def chunks(a):
        a = a.reshape(B, H, nc, ML_CHUNK, *a.shape[3:])
        return jnp.moveaxis(a, 2, 0)

    tril = jnp.tril(jnp.ones((ML_CHUNK, ML_CHUNK), bool))

    def step(carry, xs):
        C, n, m = carry
        qc, kc, vc, ic, lfc = xs
        b = jnp.cumsum(lfc, axis=-1)
        b_last = b[..., -1]
        dmat = jnp.where(tril, b[..., :, None] - b[..., None, :] + ic[..., None, :], -jnp.inf)
        inter = b + m[..., None]
        m_t = jnp.maximum(inter, dmat.max(-1))
        s = jnp.einsum('bhtd,bhsd->bhts', qc, kc) * jnp.exp(dmat - m_t[..., None])
        inter_w = jnp.exp(inter - m_t)
        num = jnp.einsum('bhts,bhsd->bhtd', s, vc) + inter_w[..., None] * jnp.einsum('bhvk,bhtk->bhtv', C, qc)
        den = s.sum(-1) + inter_w * jnp.einsum('bhk,bhtk->bht', n, qc)
        h = num / jnp.maximum(jnp.abs(den), jnp.exp(-m_t))[..., None]
        g = b_last[..., None] - b + ic
        m_new = jnp.maximum(b_last + m, g.max(-1))
        wg = jnp.exp(g - m_new[..., None])
        decay = jnp.exp(b_last + m - m_new)
        C_new = decay[..., None, None] * C + jnp.einsum('bhs,bhsv,bhsk->bhvk', wg, vc, kc)
        n_new = decay[..., None] * n + jnp.einsum('bhs,bhsk->bhk', wg, kc)
        return (C_new, n_new, m_new), h

    init = (jnp.zeros((B, H, d, d), F32), jnp.zeros((B, H, d), F32), jnp.zeros((B, H), F32))
    _, hs = lax.scan(step, init, (chunks(q), chunks(k), chunks(v), chunks(ig), chunks(lf)))
    return jnp.moveaxis(hs, 0, 2).reshape(B, H, L, d)


def hybrid_mixer(x, w_in, b_in, hy_conv_w, hy_conv_b, hy_filt_w1, hy_filt_b1, hy_filt_w2,
                 hy_filt_b2, hy_filt_w3, hy_filt_freq, hy_skip, hy_norm_w, ml_conv_w,
                 ml_conv_b, ml_norm_w, w_out, b_out):
    B, L, _ = x.shape
    proj = x @ w_in + b_in
    o1 = 3 * D_HY
    o2 = o1 + 2 * D_ML
    o3 = o2 + D_ML
    o4 = o3 + D_ML
    hy_u, ml_qk, ml_v, ml_o, ml_g = (proj[..., :o1], proj[..., o1:o2], proj[..., o2:o3],
                                     proj[..., o3:o4], proj[..., o4:])

    hy_u = centred_conv(hy_u, hy_conv_w, hy_conv_b).astype(F32)
    v, x1, x2 = jnp.split(hy_u, 3, axis=-1)
    kf = hyena_filter_spectrum(hy_filt_w1, hy_filt_b1, hy_filt_w2, hy_filt_b2, hy_filt_w3,
                               hy_filt_freq, L)
    skip = hy_skip.astype(F32)
    z = x1 * (long_conv(v, kf[:, 0]) + skip[0] * v)
    z = x2 * (long_conv(z, kf[:, 1]) + skip[1] * z)
    y_hy = group_norm(z, hy_norm_w.astype(F32), HY_GROUPS).astype(x.dtype)

    qk = jax.nn.silu(centred_conv(ml_qk, ml_conv_w, ml_conv_b))
    q, k = jnp.split(qk, 2, axis=-1)

    def to_heads(a):
        return a.reshape(B, L, ML_HEADS, ML_HEAD_DIM).transpose(0, 2, 1, 3).astype(F32)

    q, k, vm = to_heads(q), to_heads(k) * (ML_HEAD_DIM ** -0.5), to_heads(ml_v)
    gates = ml_g.astype(F32).reshape(B, L, 2, 2, ML_HEADS).transpose(2, 3, 0, 4, 1)
    h_f = mlstm_chunkwise(q, k, vm, gates[0, 0], gates[0, 1])

    def rev(a):
        return jnp.flip(a, axis=2)

    h_b = rev(mlstm_chunkwise(rev(q), rev(k), rev(vm), rev(gates[1, 0]), rev(gates[1, 1])))
    h = (h_f + h_b).transpose(0, 2, 1, 3).reshape(B, L, D_ML)
    y_ml = (group_norm(h, ml_norm_w.astype(F32), ML_HEADS) * jax.nn.sigmoid(ml_o.astype(F32))).astype(x.dtype)

    return jnp.concatenate([y_hy, y_ml], axis=-1) @ w_out + b_out


def moe_ffn(x, router_w, router_b, w_gu, b_gu, w_down, b_down):
    B, L, D = x.shape
    T = B * L
    TK = T * TOP_K
    xt = x.reshape(T, D)
    logits = (xt @ router_w + router_b).astype(F32)
    top_v, top_i = lax.top_k(logits, TOP_K)
    gates = jax.nn.softmax(top_v, axis=-1)
    e_flat = top_i.reshape(-1)
    tok_flat = jnp.arange(TK) // TOP_K
    order = jnp.argsort(e_flat)
    e_sorted = e_flat[order]
    tok_sorted = tok_flat[order]
    counts = jnp.bincount(e_flat, length=N_EXPERTS)
    starts = jnp.cumsum(counts) - counts
    padded = (counts + MOE_BLOCK - 1) // MOE_BLOCK * MOE_BLOCK
    pstarts = jnp.cumsum(padded) - padded
    dest = pstarts[e_sorted] + (jnp.arange(TK) - starts[e_sorted])
    n_blocks = (TK + MOE_BLOCK - 1) // MOE_BLOCK + N_EXPERTS
    n_rows = n_blocks * MOE_BLOCK
    buf = jnp.zeros((n_rows, D), x.dtype).at[dest].set(xt[tok_sorted])
    block_e = jnp.clip(jnp.searchsorted(pstarts + padded, jnp.arange(n_blocks) * MOE_BLOCK,
                                        side='right'), 0, N_EXPERTS - 1)

    def expert_block(args):
        xb, e = args
        hgu = xb @ w_gu[e] + b_gu[e]
        gate, up = hgu[:, :D_FF], hgu[:, D_FF:]
        gate = jnp.minimum(gate, SWIGLU_LIMIT)
        up = jnp.clip(up, -SWIGLU_LIMIT, SWIGLU_LIMIT)
        act = (up + 1.0) * (gate * jax.nn.sigmoid(SWIGLU_ALPHA * gate))
        return act @ w_down[e] + b_down[e]

    out_buf = lax.map(expert_block, (buf.reshape(n_blocks, MOE_BLOCK, D), block_e)).reshape(n_rows, D)
    y_assign = out_buf[dest] * gates.reshape(-1)[order][:, None].astype(x.dtype)
    y = jax.ops.segment_sum(y_assign, tok_sorted, num_segments=T)
    return y.reshape(B, L, D)


def setup_inputs(seed: int = 0) -> dict:
    key = jax.random.key(seed)
    ks = jax.random.split(key, 32)

    def nrm(k, shape, s):
        return jax.random.normal(k, shape, F32) * s

    x = nrm(ks[0], (BATCH, SEQ, D_MODEL), 1.0)
    w_in = nrm(ks[1], (DEPTH, D_MODEL, D_IN), D_MODEL ** -0.5)
    b_main = nrm(ks[2], (DEPTH, D_IN - N_GATE_COLS), 0.02)
    ig_bias = nrm(ks[3], (DEPTH, 2, 1, ML_HEADS), 0.1)
    fg_bias = jnp.linspace(3.0, 6.0, ML_HEADS, dtype=F32)[None, None, None, :] + nrm(ks[4], (DEPTH, 2, 1, ML_HEADS), 0.1)
    gate_bias = jnp.concatenate([ig_bias, fg_bias], axis=2).reshape(DEPTH, N_GATE_COLS)
    b_in = jnp.concatenate([b_main, gate_bias], axis=-1)
    return {
        'x': x,
        'w_in': w_in,
        'b_in': b_in,
        'hy_conv_w': nrm(ks[5], (DEPTH, HY_SHORT, 3 * D_HY), HY_SHORT ** -0.5),
        'hy_conv_b': nrm(ks[6], (DEPTH, 3 * D_HY), 0.02),
        'hy_filt_w1': nrm(ks[7], (DEPTH, HY_EMB, HY_FILT_HID), HY_EMB ** -0.5),
        'hy_filt_b1': nrm(ks[8], (DEPTH, HY_FILT_HID), 0.02),
        'hy_filt_w2': nrm(ks[9], (DEPTH, HY_FILT_HID, HY_FILT_HID), HY_FILT_HID ** -0.5),
        'hy_filt_b2': nrm(ks[10], (DEPTH, HY_FILT_HID), 0.02),
        'hy_filt_w3': nrm(ks[11], (DEPTH, HY_FILT_HID, HY_ORDER * 2 * D_HY), HY_FILT_HID ** -0.5),
        'hy_filt_freq': 1.0 + nrm(ks[12], (DEPTH, 2, HY_FILT_HID), 0.1),
        'hy_skip': nrm(ks[13], (DEPTH, HY_ORDER, D_HY), 1.0),
        'hy_norm_w': 1.0 + nrm(ks[14], (DEPTH, D_HY), 0.02),
        'ml_conv_w': nrm(ks[15], (DEPTH, ML_SHORT, 2 * D_ML), ML_SHORT ** -0.5),
        'ml_conv_b': nrm(ks[16], (DEPTH, 2 * D_ML), 0.02),
        'ml_norm_w': 1.0 + nrm(ks[17], (DEPTH, D_ML), 0.02),
        'w_out': nrm(ks[18], (DEPTH, D_MIX, D_MODEL), D_MIX ** -0.5 * DN_BETA),
        'b_out': nrm(ks[19], (DEPTH, D_MODEL), 0.02),
        'ln1_g': 1.0 + nrm(ks[20], (DEPTH, D_MODEL), 0.02),
        'ln1_b': nrm(ks[21], (DEPTH, D_MODEL), 0.02),
        'router_w': nrm(ks[22], (DEPTH, D_MODEL, N_EXPERTS), D_MODEL ** -0.5),
        'router_b': nrm(ks[23], (DEPTH, N_EXPERTS), 0.01),
        'w_gu': nrm(ks[24], (DEPTH, N_EXPERTS, D_MODEL, 2 * D_FF), D_MODEL ** -0.5),
        'b_gu': nrm(ks[25], (DEPTH, N_EXPERTS, 2 * D_FF), 0.02),
        'w_down': nrm(ks[26], (DEPTH, N_EXPERTS, D_FF, D_MODEL), D_FF ** -0.5 * DN_BETA),
        'b_down': nrm(ks[27], (DEPTH, N_EXPERTS, D_MODEL), 0.02),
        'ln2_g': 1.0 + nrm(ks[28], (DEPTH, D_MODEL), 0.02),
        'ln2_b': nrm(ks[29], (DEPTH, D_MODEL), 0.02),
    }


def reference(x, w_in, b_in, hy_conv_w, hy_conv_b, hy_filt_w1, hy_filt_b1, hy_filt_w2,
              hy_filt_b2, hy_filt_w3, hy_filt_freq, hy_skip, hy_norm_w, ml_conv_w, ml_conv_b,
              ml_norm_w, w_out, b_out, ln1_g, ln1_b, router_w, router_b, w_gu, b_gu, w_down,
              b_down, ln2_g, ln2_b):
    for l in range(DEPTH):
        mix = hybrid_mixer(x, w_in[l], b_in[l], hy_conv_w[l], hy_conv_b[l], hy_filt_w1[l],
                           hy_filt_b1[l], hy_filt_w2[l], hy_filt_b2[l], hy_filt_w3[l],
                           hy_filt_freq[l], hy_skip[l], hy_norm_w[l], ml_conv_w[l], ml_conv_b[l],
                           ml_norm_w[l], w_out[l], b_out[l])
        x = layer_norm(DN_ALPHA * x + mix, ln1_g[l], ln1_b[l])
        ff = moe_ffn(x, router_w[l], router_b[l], w_gu[l], b_gu[l], w_down[l], b_down[l])
        x = layer_norm(DN_ALPHA * x + ff, ln2_g[l], ln2_b[l])
    return x
```

```python
import numpy as np
import ml_dtypes
from contextlib import ExitStack
import concourse.bass as bass
import concourse.mybir as mybir
from concourse.bass_utils import run_bass_kernel_spmd

F32 = mybir.dt.float32
BF16 = mybir.dt.bfloat16
ALU = mybir.AluOpType
AF = mybir.ActivationFunctionType
AX = mybir.AxisListType

CELL = 512
SB_BASE = 16896
SB_END = 229344


class View:
    __slots__ = ("ap", "space", "lo", "hi")

    def __init__(self, ap, space, lo, hi):
        self.ap, self.space, self.lo, self.hi = ap, space, lo, hi


class Buf:
    def __init__(self, h, shape, es, space, base):
        self.h, self.shape, self.es, self.space, self.base = h, list(shape), es, space, base
        st = [1] * len(shape)
        for i in range(len(shape) - 2, 0, -1):
            st[i] = st[i + 1] * shape[i + 1]
        self.st = st

    def __getitem__(self, idx):
        if not isinstance(idx, tuple):
            idx = (idx,)
        ap = self.h[idx]
        lo = 0
        hi = 0
        for d in range(1, len(self.shape)):
            n = self.shape[d]
            if d < len(idx):
                ix = idx[d]
                if isinstance(ix, slice):
                    a = 0 if ix.start is None else ix.start
                    b = n if ix.stop is None else ix.stop
                else:
                    a, b = ix, ix + 1
            else:
                a, b = 0, n
            lo += a * self.st[d]
            hi += (b - 1) * self.st[d]
        hi += 1
        blo = self.base + lo * self.es
        bhi = self.base + hi * self.es
        if self.space == "ps":
            return View(ap, self.space, self.base // 2048, self.base // 2048 + 1)
        return View(ap, self.space, blo // CELL, (bhi + CELL - 1) // CELL)


class DBuf:
    def __init__(self, h, name, nslots=1):
        self.h, self.name, self.n = h, name, nslots

    def v(self, ap, lo=0, hi=None):
        return View(ap, "dr:" + self.name, lo, self.n if hi is None else hi)


FREEVARS = {}


class Op:
    __slots__ = ("eng", "fn", "R", "W", "key", "deps", "signal", "sig", "idx")


class Sched:
    ENG = ("pe", "act", "dve", "pool", "sp")

    def __init__(self, nc):
        self.nc = nc
        self.ops = []
        self.psum_i = 0

    def op(self, eng, fn, R=(), W=(), key=None):
        o = Op()
        o.eng, o.fn, o.R, o.W, o.key = eng, fn, list(R), list(W), key
        o.deps, o.signal, o.sig, o.idx = None, False, None, len(self.ops)
        if hasattr(fn, "__code__"):
            for nm in fn.__code__.co_freevars:
                FREEVARS.setdefault(nm, set()).add(fn.__code__.co_firstlineno)
        self.ops.append(o)
        return o

    def dma(self, q, out, in_, key):
        return self.op(q, lambda e: e.dma_start(out=out.ap, in_=in_.ap), R=[in_], W=[out], key=key)

    def finalize(self, stack):
        nc = self.nc
        lastw = {}
        readers = {}
        for o in self.ops:
            deps = set()
            for v in o.R:
                for c in range(v.lo, v.hi):
                    k = (v.space, c)
                    w = lastw.get(k)
                    if w is not None:
                        deps.add(w)
            for v in o.W:
                for c in range(v.lo, v.hi):
                    k = (v.space, c)
                    w = lastw.get(k)
                    if w is not None:
                        deps.add(w)
                    r = readers.get(k)
                    if r:
                        deps.update(r)
            for v in o.R:
                for c in range(v.lo, v.hi):
                    readers.setdefault((v.space, c), set()).add(o.idx)
            for v in o.W:
                for c in range(v.lo, v.hi):
                    k = (v.space, c)
                    lastw[k] = o.idx
                    readers[k] = set()
            deps.discard(o.idx)
            latest = {}
            keep = []
            for d in deps:
                od = self.ops[d]
                if od.key is not None:
                    keep.append(d)
                else:
                    if od.eng == "pe" and o.eng == "pe" and o.key is None:
                        continue
                    if latest.get(od.eng, -1) < d:
                        latest[od.eng] = d
            keep.extend(latest.values())
            o.deps = keep
            for d in keep:
                self.ops[d].signal = True
        sems = {}

        def getsem(name):
            if name not in sems:
                sems[name] = stack.enter_context(nc.semaphore("s_" + name))
            return sems[name]

        cnt = {}
        for o in self.ops:
            if o.key is not None:
                k = "d_" + o.key
                cnt[k] = cnt.get(k, 0) + 16
                o.sig = (k, cnt[k])
            elif o.signal:
                k = "e_" + o.eng
                cnt[k] = cnt.get(k, 0) + 1
                o.sig = (k, cnt[k])
        for k in cnt:
            getsem(k)
        self.nsem = len(sems)
        engs = {"pe": nc.tensor, "act": nc.scalar, "dve": nc.vector, "pool": nc.gpsimd, "sp": nc.sync}
        seen = {e: {} for e in engs}
        for o in self.ops:
            e = engs[o.eng]
            need = {}
            sn = seen[o.eng]
            for d in o.deps:
                k, val = self.ops[d].sig
                if sn.get(k, 0) >= val:
                    continue
                if need.get(k, 0) < val:
                    need[k] = val
            for k, val in need.items():
                e.wait_ge(sems[k], val)
                sn[k] = val
            ins = o.fn(e)
            if o.sig is not None:
                ins.then_inc(sems[o.sig[0]], 16 if o.key is not None else 1)
        for k, val in cnt.items():
            if k.startswith("d_"):
                nc.sync.wait_ge(sems[k], val)


D = 2048
L = 2048
NT = L // 128
OWN = 1024
NOT_ = OWN // 128
DH = 1024
DIN = 7200
NE = 32
CAP = 256
LN_EPS = 1e-5
DN_ALPHA = 2.0 ** 0.25
PI = float(np.pi)


class Ctx:
    pass


def build(stage=99, ne=NE, dbg=()):
    nc = bass.Bass("TRN2", target_bir_lowering=False)
    S = Sched(nc)
    C = Ctx()
    C.nc, C.S = nc, S
    stack = ExitStack()
    C.stack = stack

    def din(name, shape, dt=F32):
        return nc.dram_tensor(name, list(shape), dt, kind="ExternalInput")

    def dscr(name, shape, dt, nslots=1):
        kind = "ExternalOutput" if name in dbg else "Internal"
        return DBuf(nc.dram_tensor(name, list(shape), dt, kind=kind), name, nslots)

    C.din, C.dscr = din, dscr
    C.sb_names = 0

    def sb(shape, dt, off):
        es = 4 if dt == F32 else 2
        n = 1
        for x in shape[1:]:
            n *= x
        assert off % 32 == 0 and off >= 0
        assert SB_BASE + off + n * es <= SB_END, ("sbuf overflow", shape, off)
        C.sb_names += 1
        h = nc.alloc_sbuf_tensor_at("t%d" % C.sb_names, list(shape), dt, offset=SB_BASE + off)
        return Buf(h, shape, es, "sb", SB_BASE + off)

    C.sb = sb
    C.ps = []
    C.psb = []
    for i in range(8):
        h = nc.alloc_psum_tensor("ps%d" % i, [128, 512], F32)
        C.ps.append(Buf(h, [128, 512], 4, "ps", i * 2048))
        C.psb.append(Buf(h.bitcast(BF16), [128, 1024], 2, "ps", i * 2048))
    C.psr = 0
    C.ps_hold = set()

    C.dbg = dbg
    try:
        from_phases(C, stage, ne, dbg)
    except StopBuild:
        pass
    S.finalize(stack)
    stack.close()
    return nc


def nextps(C):
    while True:
        C.psr = (C.psr + 1) % 8
        if C.psr not in C.ps_hold:
            return C.psr


def ext_inputs(C, ne, stage=99):
    din = C.din
    I = Ctx()
    I.x = din("x", [L, D])
    I.w_in = din("w_in", [D, DIN])
    I.convw = din("convw", [128, 40, 5])
    I.bias_tok = din("bias_tok", [128, 2080])
    I.idb = din("idb", [128, 128], BF16)
    I.idf = din("idf", [128, 128])
    I.zT = din("zT", [33, L])
    I.decay = din("decay", [L, DH])
    I.tabA = din("tabA", [16, 128, 2, 16, 128], BF16)
    I.tabB = din("tabB", [16, 128, 2, 16, 128], BF16)
    I.fw1 = din("fw1", [33, 64])
    I.fw2 = din("fw2", [64, 64])
    I.fw3 = din("fw3", [64, 4096])
    I.fsm = din("fsm", [64, 4])
    I.skipb = din("skipb", [128, 2, DH])
    I.hnw = din("hnw", [128, DH])
    I.lagmask = din("lagmask", [128, 2])
    I.tri = din("tri", [128, 2, 128])
    I.mnw = din("mnw", [128, 1024])
    if stage < 4:
        return I
    I.w_out = din("w_out", [2048, D])
    I.boutb = din("boutb", [128, D])
    I.ln1g = din("ln1g", [128, D])
    I.ln1b = din("ln1b", [128, D])
    I.ln2g = din("ln2g", [128, D])
    I.ln2b = din("ln2b", [128, D])
    I.router_w = din("router_w", [D, NE])
    I.rbb = din("rbb", [128, NE])
    I.sutri = din("sutri", [128, 128])
    I.iota = din("iota", [128, CAP])
    if stage < 5:
        return I
    I.w_gu = din("w_gu", [ne, D, 4096])
    I.w_down = din("w_down", [ne, 2048, D])
    I.bgu = din("bgu", [ne, 128, 32])
    I.bdown = din("bdown", [NE, D])
    I.out = C.nc.dram_tensor("out", [OWN, D], F32, kind="ExternalOutput")
    return I


def phase_ab(C, I, stage):
    nc, S, sb = C.nc, C.S, C.sb
    C.idb = sb([128, 128], BF16, 0)
    C.idf = sb([128, 128], F32, 512)
    C.convw = sb([128, 40, 5], F32, 1024)
    C.gates = sb([128, 16, 32], F32, 2048)
    S.dma("sp", C.idb[:, :], DBuf(I.idb, "idb").v(I.idb.ap()), "c0")
    S.dma("sp", C.idf[:, :], DBuf(I.idf, "idf").v(I.idf.ap()), "c1")
    S.dma("sp", C.convw[:, :, :], DBuf(I.convw, "convw").v(I.convw.ap()), "c2")
    C.hyu = C.dscr("hyu", [3, L, DH], BF16, 6)
    C.qT = C.dscr("qT", [1024, L], BF16, 8)
    C.kT = C.dscr("kT", [1024, L], BF16, 8)
    C.ktok = C.dscr("ktok", [L, 1024], BF16, 2)
    C.vtok = C.dscr("vtok", [L, 1024], BF16, 2)
    C.osig = C.dscr("osig", [OWN, 1024], F32, 2)
    P0 = 8192
    xT = sb([128, 16, L], BF16, P0)
    wblk = [sb([128, 16, 512], BF16, P0 + 65536 + i * 16384) for i in range(2)]
    Q = P0 + 65536 + 32768
    xb = [sb([128, D], BF16, Q + i * 4096) for i in range(2)]
    u = [sb([128, L], F32, Q + i * 8192) for i in range(2)]
    cv = [sb([128, L], F32, Q + 16384 + i * 8192) for i in range(2)]
    cvb = [sb([128, L], BF16, Q + 32768 + i * 4096) for i in range(4)]
    stg = sb([128, 16, 512], BF16, Q + 49152)
    stgf = sb([128, 8, 512], F32, Q + 65536)
    btok = sb([128, 2080], F32, Q + 81920)
    tmpf = [sb([128, 512], F32, Q + 90624 + i * 2048) for i in range(2)]
    X = DBuf(I.x, "x")
    W = DBuf(I.w_in, "w_in")
    S.dma("sp", btok[:, :], DBuf(I.bias_tok, "bias_tok").v(I.bias_tok.ap()), "c3")
    for tt in range(NT):
        b = xb[tt % 2]
        S.dma("pool", b[:, :], X.v(I.x.ap()[tt * 128:(tt + 1) * 128, :]), "xb%d" % (tt % 2))
        for hb in range(2):
            pi = nextps(C)
            pb = C.psb[pi]
            for j in range(8):
                dt_ = hb * 8 + j
                S.op("pe", lambda e, pb=pb, b=b, j=j, dt_=dt_: e.transpose(
                    pb[:, j * 128:(j + 1) * 128].ap, b[:, dt_ * 128:(dt_ + 1) * 128].ap, C.idb[:, :].ap),
                    R=[b[:, dt_ * 128:(dt_ + 1) * 128], C.idb[:, :]], W=[pb[:, j * 128:(j + 1) * 128]])
            src = pb[:, :]
            dst = xT[:, hb * 8:(hb + 1) * 8, tt * 128:(tt + 1) * 128]
            eng = "dve" if hb == 0 else "act"
            if eng == "dve":
                S.op("dve", lambda e, src=src, dst=dst: e.tensor_copy(
                    out=dst.ap, in_=src.ap.rearrange("p (a b) -> p a b", a=8)), R=[src], W=[dst])
            else:
                S.op("act", lambda e, src=src, dst=dst: e.copy(
                    out=dst.ap, in_=src.ap.rearrange("p (a b) -> p a b", a=8)), R=[src], W=[dst])

    def load_w(c0, n, slot):
        wv = wblk[slot][:, :, 0:n]
        S.dma("pool", wv, W.v(I.w_in.ap()[:, c0:c0 + n].rearrange("(kt p) c -> p kt c", p=128)),
              "wblk%d" % slot)

    blk = 0
    for jb in range(10):
        slot = blk % 2
        blk += 1
        load_w(jb * 512, 512, slot)
        wb = wblk[slot]
        for m in range(4):
            ct = jb * 4 + m
            uu = u[ct % 2]
            cc = cv[ct % 2]
            for tb in range(4):
                pi = nextps(C)
                pv = C.ps[pi][:, 0:512]

                def mm(e, pv=pv, wb=wb, m=m, tb=tb):
                    for kt in range(16):
                        ins = e.matmul(pv.ap, wb[:, kt, m * 128:(m + 1) * 128].ap,
                                       xT[:, kt, tb * 512:(tb + 1) * 512].ap, start=(kt == 0), stop=(kt == 15))
                    return ins
                S.op("pe", mm, R=[wb[:, :, :], xT[:, :, tb * 512:(tb + 1) * 512]], W=[pv])
                uv = uu[:, tb * 512:(tb + 1) * 512]
                S.op("act", lambda e, pv=pv, uv=uv, ct=ct: e.activation(
                    out=uv.ap, in_=pv.ap, func=AF.Identity, bias=C.convw[:, ct, 4:5].ap),
                    R=[pv, C.convw[:, :, :]], W=[uv])
            S.op("dve", lambda e, uu=uu, cc=cc, ct=ct: e.tensor_scalar(
                cc[:, :].ap, uu[:, :].ap, C.convw[:, ct, 1:2].ap, C.convw[:, ct, 3:4].ap, ALU.mult, ALU.add),
                R=[uu[:, :], C.convw[:, :, :]], W=[cc[:, :]])
            S.op("dve", lambda e, uu=uu, cc=cc, ct=ct: e.scalar_tensor_tensor(
                cc[:, 1:L].ap, uu[:, 0:L - 1].ap, C.convw[:, ct, 0:1].ap, cc[:, 1:L].ap, ALU.mult, ALU.add),
                R=[uu[:, :], cc[:, :], C.convw[:, :, :]], W=[cc[:, :]])
            S.op("dve", lambda e, uu=uu, cc=cc, ct=ct: e.scalar_tensor_tensor(
                cc[:, 0:L - 1].ap, uu[:, 1:L].ap, C.convw[:, ct, 2:3].ap, cc[:, 0:L - 1].ap, ALU.mult, ALU.add),
                R=[uu[:, :], cc[:, :], C.convw[:, :, :]], W=[cc[:, :]])
            cb_ = cvb[m]
            if jb < 6:
                S.op("act", lambda e, cc=cc, cb_=cb_: e.copy(out=cb_[:, :].ap, in_=cc[:, :].ap),
                     R=[cc[:, :]], W=[cb_[:, :]])
            elif jb < 8:
                S.op("act", lambda e, cc=cc, cb_=cb_: e.activation(out=cb_[:, :].ap, in_=cc[:, :].ap, func=AF.Silu),
                     R=[cc[:, :]], W=[cb_[:, :]])
                hh = (jb - 6) * 4 + m
                S.dma("sp", C.qT.v(C.qT.h.ap()[hh * 128:(hh + 1) * 128, :], hh, hh + 1), cb_[:, :], "qT%d" % m)
            else:
                S.op("act", lambda e, cc=cc: e.activation(out=cc[:, :].ap, in_=cc[:, :].ap, func=AF.Silu),
                     R=[cc[:, :]], W=[cc[:, :]])
                S.op("dve", lambda e, cc=cc, cb_=cb_: e.tensor_scalar(
                    cb_[:, :].ap, cc[:, :].ap, float(128 ** -0.5), None, ALU.mult),
                    R=[cc[:, :]], W=[cb_[:, :]])
                hh = (jb - 8) * 4 + m
                S.dma("sp", C.kT.v(C.kT.h.ap()[hh * 128:(hh + 1) * 128, :], hh, hh + 1), cb_[:, :], "kT%d" % m)
        if jb < 6 or jb >= 8:
            for tt in range(NT):
                pi = nextps(C)
                pb = C.psb[pi]
                for m in range(4):
                    S.op("pe", lambda e, pb=pb, m=m, tt=tt: e.transpose(
                        pb[:, m * 128:(m + 1) * 128].ap, cvb[m][:, tt * 128:(tt + 1) * 128].ap, C.idb[:, :].ap),
                        R=[cvb[m][:, tt * 128:(tt + 1) * 128], C.idb[:, :]], W=[pb[:, m * 128:(m + 1) * 128]])
                sv = stg[:, tt, :]
                if tt % 2 == 0:
                    S.op("dve", lambda e, pb=pb, sv=sv: e.tensor_copy(out=sv.ap, in_=pb[:, 0:512].ap),
                         R=[pb[:, 0:512]], W=[sv])
                else:
                    S.op("act", lambda e, pb=pb, sv=sv: e.copy(out=sv.ap, in_=pb[:, 0:512].ap),
                         R=[pb[:, 0:512]], W=[sv])
            if jb < 6:
                w_, cbk = jb // 2, jb % 2
                dst = C.hyu.v(C.hyu.h.ap()[w_, :, cbk * 512:(cbk + 1) * 512].rearrange("(tt p) c -> p tt c", p=128),
                              jb, jb + 1)
            else:
                cbk = jb - 8
                dst = C.ktok.v(C.ktok.h.ap()[:, cbk * 512:(cbk + 1) * 512].rearrange("(tt p) c -> p tt c", p=128),
                               cbk, cbk + 1)
            S.dma("sp", dst, stg[:, :, :], "stg")
    for jb in range(5):
        slot = blk % 2
        blk += 1
        c0 = 5120 + jb * 512
        n = 512 if jb < 4 else 32
        load_w(c0, n, slot)
        wb = wblk[slot]
        ntt = NT if (jb < 2 or jb == 4) else NOT_
        for tt in range(ntt):
            pi = nextps(C)
            pv = C.ps[pi][:, 0:n]

            def mm(e, pv=pv, wb=wb, tt=tt, n=n):
                for kt in range(16):
                    ins = e.matmul(pv.ap, xT[:, kt, tt * 128:(tt + 1) * 128].ap, wb[:, kt, 0:n].ap,
                                   start=(kt == 0), stop=(kt == 15))
                return ins
            S.op("pe", mm, R=[wb[:, :, :], xT[:, :, tt * 128:(tt + 1) * 128]], W=[pv])
            bv = btok[:, c0 - 5120:c0 - 5120 + n]
            if jb < 2:
                sv = stg[:, tt, :]
                S.op("dve", lambda e, pv=pv, sv=sv, bv=bv: e.tensor_tensor(sv.ap, pv.ap, bv.ap, ALU.add),
                     R=[pv, bv], W=[sv])
            elif jb < 4:
                tv = tmpf[tt % 2][:, :]
                S.op("dve", lambda e, pv=pv, tv=tv, bv=bv: e.tensor_tensor(tv.ap, pv.ap, bv.ap, ALU.add),
                     R=[pv, bv], W=[tv])
                sv = stgf[:, tt, :]
                S.op("act", lambda e, tv=tv, sv=sv: e.activation(out=sv.ap, in_=tv.ap, func=AF.Sigmoid),
                     R=[tv], W=[sv])
            else:
                gv = C.gates[:, tt, :]
                S.op("dve", lambda e, pv=pv, gv=gv, bv=bv: e.tensor_tensor(gv.ap, pv.ap, bv.ap, ALU.add),
                     R=[pv, bv], W=[gv])
        if jb < 2:
            dst = C.vtok.v(C.vtok.h.ap()[:, jb * 512:(jb + 1) * 512].rearrange("(tt p) c -> p tt c", p=128), jb, jb + 1)
            S.dma("sp", dst, stg[:, :, :], "stg")
        elif jb < 4:
            cbk = jb - 2
            dst = C.osig.v(C.osig.h.ap()[:, cbk * 512:(cbk + 1) * 512].rearrange("(tt p) c -> p tt c", p=128), cbk, cbk + 1)
            S.dma("sp", dst, stgf[:, :, :], "stgf")


def dump(C, name, view, shape, dt=F32):
    h = C.nc.dram_tensor(name, list(shape), dt, kind="ExternalOutput")
    C.S.dma("sp", DBuf(h, name).v(h.ap()), view, "dump_" + name)


class StopBuild(Exception):
    pass


def act_sin(C, out, arg, tmp1, tmp2, R, W):
    S = C.S
    S.op("act", lambda e: e.activation(out=tmp1.ap, in_=arg.ap, func=AF.Sin, scale=0.5), R=[arg], W=[tmp1])
    S.op("act", lambda e: e.activation(out=tmp2.ap, in_=arg.ap, func=AF.Abs), R=[arg], W=[tmp2])
    S.op("act", lambda e: e.activation(out=tmp2.ap, in_=tmp2.ap, func=AF.Sin, scale=-0.5, bias=C.hpi[:, :].ap[0:64]),
         R=[tmp2, C.hpi[:, :]], W=[tmp2])
    S.op("dve", lambda e: e.scalar_tensor_tensor(out.ap, tmp1.ap, 2.0, tmp2.ap, ALU.mult, ALU.mult),
         R=[tmp1, tmp2], W=[out])


def phase_c(C, I, stage):
    nc, S, sb = C.nc, C.S, C.sb
    P0 = 8192
    K1 = 1024
    C.ymix = C.dscr("ymix", [OWN, 2048], BF16, 4)
    C.hpi = sb([128, 1], F32, 4096)
    C.eps = sb([128, 1], F32, 4128)
    C.onesf = sb([128, 128], F32, 4608)
    lagm = sb([128, 2], F32, 4160)
    S.op("dve", lambda e: e.memset(C.hpi[:, :].ap, PI / 2), W=[C.hpi[:, :]])
    S.op("dve", lambda e: e.memset(C.eps[:, :].ap, LN_EPS), W=[C.eps[:, :]])
    S.op("dve", lambda e: e.memset(C.onesf[:, :].ap, 1.0), W=[C.onesf[:, :]])
    S.dma("sp", lagm[:, :], DBuf(I.lagmask, "lagmask").v(I.lagmask.ap()), "c4")
    zT = sb([33, L], F32, P0)
    h2T = sb([64, L], F32, P0 + 8 * K1)
    h1T = sb([64, L], F32, P0 + 16 * K1)
    w3 = sb([64, 4096], F32, P0 + 24 * K1)
    w1 = sb([33, 64], F32, P0 + 40 * K1)
    w2 = sb([64, 64], F32, P0 + 40 * K1 + 256)
    fsm = sb([64, 4], F32, P0 + 40 * K1 + 512)
    skipb = sb([128, 2, DH], F32, P0 + 41 * K1)
    hnw = sb([128, DH], F32, P0 + 49 * K1)
    vz = sb([128, 16, 512], BF16, P0 + 54 * K1)
    Kre = sb([128, 16, 512], BF16, P0 + 70 * K1)
    Kim = sb([128, 16, 512], BF16, P0 + 86 * K1)
    tabs = [sb([128, 2, 16, 128], BF16, P0 + (102 + 8 * i) * K1) for i in range(3)]
    Pb = sb([128, 16, 512], BF16, P0 + 126 * K1)
    Qb = sb([128, 16, 512], BF16, P0 + 142 * K1)
    kp, km = Pb, Qb
    XR = [sb([128, 512], F32, P0 + (158 + 2 * i) * K1) for i in range(2)]
    XS = [sb([128, 512], F32, P0 + (162 + 2 * i) * K1) for i in range(2)]
    T = [sb([128, 512], F32, P0 + (166 + 2 * i) * K1) for i in range(4)]
    zz = sb([128, 8, 512], F32, P0 + 158 * K1)
    x1t = [sb([128, 512], BF16, P0 + (174 + i) * K1) for i in range(3)]
    ystage = sb([128, 8, 512], BF16, P0 + 177 * K1)
    dec = [sb([128, 512], F32, P0 + 2 * i * K1) for i in range(2)]
    absb = [sb([128, 512], F32, P0 + (4 + 2 * i) * K1) for i in range(2)]
    sc = sb([128, 512], F32, P0 + 16 * K1)
    kft = [sb([128, 512], F32, P0 + (18 + 2 * i) * K1) for i in range(2)]
    gst = sb([128, 4, 6], F32, P0 + 185 * K1)
    gag = sb([128, 4, 2], F32, P0 + 185 * K1 + 128)
    grs = sb([128, 4], F32, P0 + 185 * K1 + 192)
    yn = sb([128, 512], F32, P0 + 186 * K1)

    def ld(dst, h, name, key, q="sp"):
        S.dma(q, dst, DBuf(h, name).v(h.ap()), key)
    ld(zT[:, :], I.zT, "zT", "c5")
    ld(w1[:, :], I.fw1, "fw1", "c6")
    ld(w2[:, :], I.fw2, "fw2", "c7")
    ld(w3[:, :], I.fw3, "fw3", "c8")
    ld(fsm[:, :], I.fsm, "fsm", "c9")
    ld(skipb[:, :, :], I.skipb, "skipb", "c10")
    ld(hnw[:, :], I.hnw, "hnw", "c11")
    S.op("dve", lambda e: e.tensor_scalar(skipb[:, :, :].ap, skipb[:, :, :].ap, 1.0 / 2048.0, None, ALU.mult),
         R=[skipb[:, :, :]], W=[skipb[:, :, :]])
    for layer in range(2):
        src = zT if layer == 0 else h1T
        dstT = h1T if layer == 0 else h2T
        ww = w1 if layer == 0 else w2
        for tb in range(4):
            pi = nextps(C)
            pv = C.ps[pi][0:64, 0:512]
            sv = src[:, tb * 512:(tb + 1) * 512]
            S.op("pe", lambda e, pv=pv, ww=ww, sv=sv: e.matmul(pv.ap, ww[:, :].ap, sv.ap, start=True, stop=True),
                 R=[ww[:, :], sv], W=[pv])
            a_ = T[0][0:64, :]
            S.op("dve", lambda e, pv=pv, a_=a_, layer=layer: e.tensor_scalar(
                a_.ap, pv.ap, fsm[:, layer:layer + 1].ap, fsm[:, 2 + layer:3 + layer].ap, ALU.add, ALU.mult),
                R=[pv, fsm[:, :]], W=[a_])
            act_sin(C, dstT[:, tb * 512:(tb + 1) * 512], a_, T[1][0:64, :], T[2][0:64, :], None, None)

    if "c1" in C.dbg:
        dump(C, "h2T_o", h2T[:, :], [64, L])
        dump(C, "h1T_o", h1T[:, :], [64, L])
        raise StopBuild()
    TA = DBuf(I.tabA, "tabA")
    TB = DBuf(I.tabB, "tabB")
    DEC = DBuf(I.decay, "decay")
    HY = C.hyu
    tabi = [0]

    def load_tab(Tb, h, idx):
        slot = tabi[0] % 3
        tabi[0] += 1
        S.dma("sp", tabs[slot][:, :, :, :], Tb.v(h.ap()[idx]), "tab%d" % slot)
        return tabs[slot]

    def fwd_dft(ft, src):
        tb_ = load_tab(TA, I.tabA, ft)
        pis = []
        for cs in range(2):
            pi = nextps(C)
            pis.append(pi)
            pv = C.ps[pi][:, 0:512]

            def mm(e, pv=pv, tb_=tb_, cs=cs):
                for st in range(16):
                    ins = e.matmul(pv.ap, tb_[:, cs, st, :].ap, src[:, st, :].ap, start=(st == 0), stop=(st == 15))
                return ins
            S.op("pe", mm, R=[tb_[:, cs, :, :], src[:, :, :]], W=[pv])
        return pis

    for cb in range(2):
        cs0 = cb * 512
        for o in range(2):
            hold = nextps(C)
            C.ps_hold.add(hold)
            Sps = C.ps[hold][:, 0:512]
            for lt in range(16):
                dv = dec[lt % 2][:, :]
                S.dma("sp", dv, DEC.v(I.decay.ap()[lt * 128:(lt + 1) * 128, cs0:cs0 + 512]), "dec%d" % (lt % 2))
                kk = []
                for d_ in range(2):
                    pi = nextps(C)
                    pv = C.ps[pi][:, 0:512]
                    col = o * 2048 + d_ * 1024 + cs0
                    S.op("pe", lambda e, pv=pv, lt=lt, col=col: e.matmul(
                        pv.ap, h2T[:, lt * 128:(lt + 1) * 128].ap, w3[:, col:col + 512].ap, start=True, stop=True),
                        R=[h2T[:, :], w3[:, :]], W=[pv])
                    kv = kft[d_][:, :]
                    S.op("dve", lambda e, pv=pv, kv=kv, dv=dv: e.tensor_tensor(kv.ap, pv.ap, dv.ap, ALU.mult),
                         R=[pv, dv], W=[kv])
                    if lt == 0:
                        S.op("dve", lambda e, kv=kv, d_=d_: e.tensor_scalar(
                            kv.ap, kv.ap, lagm[:, d_:d_ + 1].ap, None, ALU.mult), R=[kv, lagm[:, :]], W=[kv])
                    av = absb[d_][:, :]
                    S.op("act", lambda e, kv=kv, av=av: e.activation(out=av.ap, in_=kv.ap, func=AF.Abs), R=[kv], W=[av])
                    S.op("pe", lambda e, av=av, lt=lt, d_=d_, Sps=Sps: e.matmul(
                        Sps.ap, C.onesf[:, :].ap, av.ap, start=(lt == 0 and d_ == 0), stop=(lt == 15 and d_ == 1)),
                        R=[av, C.onesf[:, :]], W=[Sps])
                    kk.append(kv)
                S.op("dve", lambda e, lt=lt, kk=kk: e.tensor_tensor(kp[:, lt, :].ap, kk[0].ap, kk[1].ap, ALU.add),
                     R=[kk[0], kk[1]], W=[kp[:, lt, :]])
                S.op("dve", lambda e, lt=lt, kk=kk: e.tensor_tensor(km[:, lt, :].ap, kk[1].ap, kk[0].ap, ALU.subtract),
                     R=[kk[0], kk[1]], W=[km[:, lt, :]])
            S.op("dve", lambda e, Sps=Sps: e.tensor_scalar(sc[:, :].ap, Sps.ap, 2048.0, None, ALU.mult), R=[Sps], W=[sc[:, :]])
            S.op("dve", lambda e: e.reciprocal(out=sc[:, :].ap, in_=sc[:, :].ap), R=[sc[:, :]], W=[sc[:, :]])
            C.ps_hold.discard(hold)
            for ft in range(16):
                tb_ = load_tab(TA, I.tabA, ft)
                for cs in range(2):
                    pi = nextps(C)
                    pv = C.ps[pi][:, 0:512]
                    srck = kp if cs == 0 else km

                    def mm(e, pv=pv, tb_=tb_, cs=cs, srck=srck):
                        for st in range(16):
                            ins = e.matmul(pv.ap, tb_[:, cs, st, :].ap, srck[:, st, :].ap, start=(st == 0), stop=(st == 15))
                        return ins
                    S.op("pe", mm, R=[tb_[:, cs, :, :], srck[:, :, :]], W=[pv])
                    if cs == 0:
                        tv = T[0][:, :]
                        S.op("dve", lambda e, pv=pv, tv=tv: e.tensor_tensor(tv.ap, pv.ap, sc[:, :].ap, ALU.mult),
                             R=[pv, sc[:, :]], W=[tv])
                        S.op("pool", lambda e, tv=tv, ft=ft, o=o, cs0=cs0: e.tensor_tensor(
                            Kre[:, ft, :].ap, tv.ap, skipb[:, o, cs0:cs0 + 512].ap, ALU.add),
                            R=[tv, skipb[:, :, :]], W=[Kre[:, ft, :]])
                    else:
                        S.op("dve", lambda e, pv=pv, ft=ft: e.tensor_tensor(Kim[:, ft, :].ap, pv.ap, sc[:, :].ap, ALU.mult),
                             R=[pv, sc[:, :]], W=[Kim[:, ft, :]])
            if "spec%d" % o in C.dbg:
                dump(C, "Kre_o", Kre[:, :, :], [128, 16, 512], BF16)
                raise StopBuild()
            if "c2" in C.dbg:
                dump(C, "kp_o", kp[:, :, :], [128, 16, 512], BF16)
                dump(C, "km_o", km[:, :, :], [128, 16, 512], BF16)
                dump(C, "Kre_o", Kre[:, :, :], [128, 16, 512], BF16)
                dump(C, "Kim_o", Kim[:, :, :], [128, 16, 512], BF16)
                dump(C, "sc_o", sc[:, :], [128, 512])
                raise StopBuild()
            if o == 0:
                S.dma("sp", vz[:, :, :], HY.v(HY.h.ap()[0, :, cs0:cs0 + 512].rearrange("(tt p) c -> p tt c", p=128),
                                             cb, cb + 1), "vz")
            for ft in range(16):
                pr, ps_ = fwd_dft(ft, vz)
                xr = XR[ft % 2][:, :]
                xs = XS[ft % 2][:, :]
                S.op("act", lambda e, pr=pr, xr=xr: e.copy(out=xr.ap, in_=C.ps[pr][:, 0:512].ap), R=[C.ps[pr][:, 0:512]], W=[xr])
                S.op("act", lambda e, ps_=ps_, xs=xs: e.copy(out=xs.ap, in_=C.ps[ps_][:, 0:512].ap), R=[C.ps[ps_][:, 0:512]], W=[xs])
                kr = Kre[:, ft, :]
                ki = Kim[:, ft, :]
                S.op("dve", lambda e, xr=xr, kr=kr: e.tensor_tensor(T[0][:, :].ap, xr.ap, kr.ap, ALU.mult), R=[xr, kr], W=[T[0][:, :]])
                S.op("dve", lambda e, xs=xs, ki=ki: e.tensor_tensor(T[1][:, :].ap, xs.ap, ki.ap, ALU.mult), R=[xs, ki], W=[T[1][:, :]])
                S.op("dve", lambda e, ft=ft: e.tensor_tensor(Pb[:, ft, :].ap, T[0][:, :].ap, T[1][:, :].ap, ALU.add),
                     R=[T[0][:, :], T[1][:, :]], W=[Pb[:, ft, :]])
                S.op("dve", lambda e, xs=xs, kr=kr: e.tensor_tensor(T[2][:, :].ap, xs.ap, kr.ap, ALU.mult), R=[xs, kr], W=[T[2][:, :]])
                S.op("pool", lambda e, xr=xr, ki=ki: e.tensor_tensor(T[3][:, :].ap, xr.ap, ki.ap, ALU.mult), R=[xr, ki], W=[T[3][:, :]])
                S.op("pool", lambda e, ft=ft: e.tensor_tensor(Qb[:, ft, :].ap, T[2][:, :].ap, T[3][:, :].ap, ALU.subtract),
                     R=[T[2][:, :], T[3][:, :]], W=[Qb[:, ft, :]])
            if "fwd%d" % o in C.dbg:
                dump(C, "P_o", Pb[:, :, :], [128, 16, 512], BF16)
                raise StopBuild()
            ntt = NT if o == 0 else NOT_
            for tt in range(ntt):
                tb_ = load_tab(TB, I.tabB, tt)
                xm = x1t[tt % 3][:, :]
                S.dma("sp", xm, HY.v(HY.h.ap()[1 + o, tt * 128:(tt + 1) * 128, cs0:cs0 + 512], 2 * (1 + o) + cb, 2 * (1 + o) + cb + 1),
                      "x1t%d" % (tt % 3))
                pi = nextps(C)
                pv = C.ps[pi][:, 0:512]

                def mm(e, pv=pv, tb_=tb_):
                    for ft in range(16):
                        e.matmul(pv.ap, tb_[:, 0, ft, :].ap, Pb[:, ft, :].ap, start=(ft == 0), stop=False)
                    for ft in range(16):
                        ins = e.matmul(pv.ap, tb_[:, 1, ft, :].ap, Qb[:, ft, :].ap, start=False, stop=(ft == 15))
                    return ins
                S.op("pe", mm, R=[tb_[:, :, :, :], Pb[:, :, :], Qb[:, :, :]], W=[pv])
                if o == 0:
                    S.op("dve", lambda e, pv=pv, xm=xm, tt=tt: e.tensor_tensor(vz[:, tt, :].ap, pv.ap, xm.ap, ALU.mult),
                         R=[pv, xm], W=[vz[:, tt, :]])
                else:
                    zv = zz[:, tt, :]
                    S.op("dve", lambda e, pv=pv, xm=xm, zv=zv: e.tensor_tensor(zv.ap, pv.ap, xm.ap, ALU.mult),
                         R=[pv, xm], W=[zv])
                    if "c4a" in C.dbg:
                        continue
                    for g in range(4):
                        S.op("dve", lambda e, g=g, zv=zv, tt=tt: e.bn_stats(gst[:, g, :].ap, zz[:, tt, g * 128:(g + 1) * 128].ap),
                             R=[zv], W=[gst[:, :, :]])
                        S.op("dve", lambda e, g=g: e.bn_aggr(gag[:, g, :].ap, gst[:, g, :].ap), R=[gst[:, :, :]], W=[gag[:, :, :]])
                    S.op("act", lambda e: e.activation(out=grs[:, :].ap, in_=gag[:, :, 1].ap, func=AF.Sqrt, bias=C.eps[:, :].ap),
                         R=[gag[:, :, :], C.eps[:, :]], W=[grs[:, :]])
                    S.op("dve", lambda e: e.reciprocal(out=grs[:, :].ap, in_=grs[:, :].ap), R=[grs[:, :]], W=[grs[:, :]])
                    for g in range(4):
                        S.op("dve", lambda e, g=g, tt=tt, zv=zv: e.tensor_scalar(
                            yn[:, g * 128:(g + 1) * 128].ap, zz[:, tt, g * 128:(g + 1) * 128].ap,
                            gag[:, g, 0:1].ap, grs[:, g:g + 1].ap, ALU.subtract, ALU.mult),
                            R=[zv, gag[:, :, :], grs[:, :]], W=[yn[:, :]])
                    S.op("pool", lambda e, tt=tt, cs0=cs0: e.tensor_tensor(ystage[:, tt, :].ap, yn[:, :].ap, hnw[:, cs0:cs0 + 512].ap, ALU.mult),
                         R=[yn[:, :], hnw[:, :]], W=[ystage[:, tt, :]])
            if "c3" in C.dbg:
                dump(C, "z_o", vz[:, :, :], [128, 16, 512], BF16)
                dump(C, "P_o", Pb[:, :, :], [128, 16, 512], BF16)
                raise StopBuild()
            if o == 1 and "c4a" in C.dbg:
                dump(C, "zz_o", zz[:, :, :], [128, 8, 512])
                raise StopBuild()
            if o == 1:
                S.dma("sp", C.ymix.v(C.ymix.h.ap()[:, cs0:cs0 + 512].rearrange("(tt p) c -> p tt c", p=128), cb, cb + 1),
                      ystage[:, :, :], "ystage")
                if "c4" in C.dbg:
                    dump(C, "zz_o", zz[:, :, :], [128, 8, 512])
                    dump(C, "ys_o", ystage[:, :, :], [128, 8, 512], BF16)
                    raise StopBuild()


def phase_d(C, I, stage):
    nc, S, sb = C.nc, C.S, C.sb
    P0 = 8192
    K1 = 1024
    tri = sb([128, 2, 128], F32, 5120)
    S.dma("sp", tri[:, :, :], DBuf(I.tri, "tri").v(I.tri.ap()), "c12")
    SP = sb([128, 16, 16], F32, P0)
    EB = sb([128, 16, 16], F32, P0 + K1)
    AA = sb([128, 16, 16], F32, P0 + 2 * K1)
    EBL = sb([128, 16, 16], F32, P0 + 3 * K1)
    REB = sb([128, 16, 16], F32, P0 + 5 * K1)
    gtmp = sb([128, 16], F32, P0 + 4 * K1)
    qTh = [sb([128, L], BF16, P0 + (8 + 4 * i) * K1) for i in range(4)]
    kTh = [sb([128, L], BF16, P0 + (24 + 4 * i) * K1) for i in range(4)]
    ktk = [sb([128, 16, 128], BF16, P0 + (40 + 4 * i) * K1) for i in range(4)]
    vau = [sb([128, 16, 129], BF16, P0 + 56 * K1 + 4608 * i) for i in range(4)]
    Cf = [sb([128, 129], F32, P0 + (76 + i) * K1) for i in range(8)]
    CTb = [sb([128, 129], BF16, P0 + 84 * K1 + 512 * i) for i in range(8)]
    stmp = [sb([128, 129], F32, P0 + (88 + i) * K1) for i in range(8)]
    vab = [sb([128, 129], BF16, P0 + 96 * K1 + 512 * i) for i in range(16)]
    stm = [sb([128, 128], BF16, P0 + 104 * K1 + 512 * i) for i in range(16)]
    dsc = [sb([128, 8], F32, P0 + 112 * K1 + 512 * i) for i in range(8)]
    hacc = sb([128, 8, 1024], F32, P0 + 120 * K1)
    osg = [sb([128, 1024], F32, P0 + (152 + 4 * i) * K1) for i in range(2)]
    mnw = sb([128, 1024], F32, P0 + 160 * K1)
    ystg = [sb([128, 1024], BF16, P0 + (164 + 2 * i) * K1) for i in range(2)]
    yn2 = sb([128, 1024], F32, P0 + 168 * K1)
    gst2 = sb([128, 8, 6], F32, P0 + 172 * K1)
    gag2 = sb([128, 8, 2], F32, P0 + 172 * K1 + 512)
    grs2 = sb([128, 8], F32, P0 + 173 * K1)
    S.dma("sp", mnw[:, :], DBuf(I.mnw, "mnw").v(I.mnw.ap()), "c13")
    gates = C.gates
    g5 = gates.h.ap().rearrange("p t (d i h) -> p t d i h", d=2, i=2)
    gall = gates[:, :, :]
    for d in range(2):
        S.op("act", lambda e, d=d: e.activation(out=SP[:, :, d * 8:(d + 1) * 8].ap, in_=g5[:, :, d, 1, :], func=AF.Exp, scale=-1.0),
             R=[gall], W=[SP[:, :, :]])
    S.op("act", lambda e: e.activation(out=SP[:, :, :].ap, in_=SP[:, :, :].ap, func=AF.Ln, bias=1.0), R=[SP[:, :, :]], W=[SP[:, :, :]])
    for c in range(16):
        pi = nextps(C)
        pv = C.ps[pi][:, 0:32]
        S.op("pe", lambda e, pv=pv, c=c: e.matmul(pv.ap[:, 0:8], tri[:, 0, :].ap, SP[:, c, 0:8].ap, start=True, stop=True),
             R=[tri[:, :, :], SP[:, :, :]], W=[pv])
        S.op("pe", lambda e, pv=pv, c=c: e.matmul(pv.ap[:, 8:16], tri[:, 1, :].ap, SP[:, c, 8:16].ap, start=True, stop=True),
             R=[tri[:, :, :], SP[:, :, :]], W=[pv])
        S.op("pe", lambda e, pv=pv, c=c: e.matmul(pv.ap[:, 16:32], C.onesf[:, :].ap, SP[:, c, :].ap, start=True, stop=True),
             R=[C.onesf[:, :], SP[:, :, :]], W=[pv])
        S.op("act", lambda e, pv=pv, c=c: e.activation(out=EB[:, c, :].ap, in_=pv.ap[:, 0:16], func=AF.Exp, scale=-1.0),
             R=[pv], W=[EB[:, c, :]])
        S.op("act", lambda e, pv=pv, c=c: e.activation(out=EBL[:, c, :].ap, in_=pv.ap[:, 16:32], func=AF.Exp, scale=-1.0),
             R=[pv], W=[EBL[:, c, :]])
        S.op("act", lambda e, pv=pv, c=c: e.activation(out=REB[:, c, :].ap, in_=pv.ap[:, 0:16], func=AF.Exp),
             R=[pv], W=[REB[:, c, :]])
        S.op("dve", lambda e, pv=pv, c=c: e.tensor_tensor(
            gtmp[:, :].ap.rearrange("p (d h) -> p d h", d=2), pv.ap[:, 0:16].rearrange("p (d h) -> p d h", d=2),
            g5[:, c, :, 0, :], ALU.add), R=[pv, gall], W=[gtmp[:, :]])
        S.op("act", lambda e, c=c: e.activation(out=AA[:, c, :].ap, in_=gtmp[:, :].ap, func=AF.Exp),
             R=[gtmp[:, :]], W=[AA[:, c, :]])

    QT, KT, KTOK, VTOK, OS = C.qT, C.kT, C.ktok, C.vtok, C.osig
    for i in range(4):
        S.op("pool", lambda e, i=i: e.memset(vau[i][:, :, 128:129].ap, 1.0), W=[vau[i][:, :, :]])
    for grp in range(2):
        for i in range(4):
            hh = grp * 4 + i
            S.dma("sp", qTh[i][:, :], QT.v(QT.h.ap()[hh * 128:(hh + 1) * 128, :], hh, hh + 1), "qTh%d" % i)
            S.dma("sp", kTh[i][:, :], KT.v(KT.h.ap()[hh * 128:(hh + 1) * 128, :], hh, hh + 1), "kTh%d" % i)
            S.dma("sp", ktk[i][:, :, :], KTOK.v(KTOK.h.ap()[:, hh * 128:(hh + 1) * 128].rearrange("(c p) k -> p c k", p=128),
                                                 hh // 4, hh // 4 + 1), "ktk%d" % i)
            S.dma("sp", vau[i][:, :, 0:128], VTOK.v(VTOK.h.ap()[:, hh * 128:(hh + 1) * 128].rearrange("(c p) k -> p c k", p=128),
                                                    hh // 4, hh // 4 + 1), "vau%d" % i)
        for ch in range(8):
            S.op("pool", lambda e, ch=ch: e.memset(Cf[ch][:, :].ap, 0.0), W=[Cf[ch][:, :]])
            S.op("pool", lambda e, ch=ch: e.memset(CTb[ch][:, :].ap, 0.0), W=[CTb[ch][:, :]])
        vai = [0] * 8
        for step in range(16):
            for ch in range(8):
                i, d = ch % 4, ch // 4
                hh = grp * 4 + i
                if d == 0:
                    if step >= 8:
                        continue
                    c, full = step, True
                else:
                    c, full = 15 - step, step >= 8
                gcol = d * 8 + hh
                vslot = ch * 2 + (vai[ch] % 2)
                vai[ch] += 1
                va = vab[vslot][:, :]
                S.op("act", lambda e, va=va, i=i, c=c, gcol=gcol: e.activation(
                    out=va.ap, in_=vau[i][:, c, :].ap, func=AF.Copy, scale=AA[:, c, gcol:gcol + 1].ap),
                    R=[vau[i][:, c, :], AA[:, c, :]], W=[va])
                cs_ = slice(c * 128, (c + 1) * 128)
                if full:
                    stv = C.ps[nextps(C)][:, 0:128]
                    S.op("pe", lambda e, stv=stv, i=i, cs_=cs_: e.matmul(stv.ap, kTh[i][:, cs_].ap, qTh[i][:, cs_].ap, start=True, stop=True),
                         R=[kTh[i][:, cs_], qTh[i][:, cs_]], W=[stv])
                last_upd = not ((d == 0 and c == 7) or (d == 1 and c == 0))
                if last_upd:
                    upv = C.ps[nextps(C)][:, 0:129]
                    S.op("pe", lambda e, upv=upv, i=i, c=c, va=va: e.matmul(upv.ap, ktk[i][:, c, :].ap, va.ap, start=True, stop=True),
                         R=[ktk[i][:, c, :], va], W=[upv])
                if full:
                    sm = stm[vslot][:, :]
                    S.op("dve", lambda e, stv=stv, sm=sm, d=d: e.tensor_tensor(sm.ap, stv.ap, tri[:, d, :].ap, ALU.mult),
                         R=[stv, tri[:, :, :]], W=[sm])
                    nv = C.ps[nextps(C)][:, 0:129]

                    def mmn(e, nv=nv, sm=sm, va=va, i=i, cs_=cs_, ch=ch):
                        e.matmul(nv.ap, sm.ap, va.ap, start=True, stop=False)
                        return e.matmul(nv.ap, qTh[i][:, cs_].ap, CTb[ch][:, :].ap, start=False, stop=True)
                    S.op("pe", mmn, R=[sm, va, qTh[i][:, cs_], CTb[ch][:, :]], W=[nv])
                if last_upd:
                    tv = stmp[ch][:, :]
                    S.op("dve", lambda e, tv=tv, ch=ch, upv=upv: e.tensor_tensor(tv.ap, Cf[ch][:, :].ap, upv.ap, ALU.add),
                         R=[Cf[ch][:, :], upv], W=[tv])
                    S.op("dve", lambda e, tv=tv, ch=ch, c=c, gcol=gcol: e.tensor_scalar(
                        Cf[ch][:, :].ap, tv.ap, EBL[:, c, gcol:gcol + 1].ap, None, ALU.mult),
                        R=[tv, EBL[:, c, :]], W=[Cf[ch][:, :]])
                    S.op("act", lambda e, tv=tv, ch=ch, c=c, gcol=gcol: e.activation(
                        out=CTb[ch][:, :].ap, in_=tv.ap, func=AF.Copy, scale=EBL[:, c, gcol:gcol + 1].ap),
                        R=[tv, EBL[:, c, :]], W=[CTb[ch][:, :]])
                if full:
                    ds = dsc[ch]
                    rebv = REB[:, c, gcol:gcol + 1]
                    S.op("act", lambda e, ds=ds, nv=nv: e.activation(out=ds[:, 0:1].ap, in_=nv.ap[:, 128:129], func=AF.Abs),
                         R=[nv], W=[ds[:, :]])
                    S.op("dve", lambda e, ds=ds, rebv=rebv: e.tensor_tensor(ds[:, 4:5].ap, ds[:, 0:1].ap, rebv.ap, ALU.max),
                         R=[ds[:, :], rebv], W=[ds[:, :]])
                    S.op("dve", lambda e, ds=ds: e.reciprocal(out=ds[:, 5:6].ap, in_=ds[:, 4:5].ap), R=[ds[:, :]], W=[ds[:, :]])
                    hv = hacc[:, c, hh * 128:(hh + 1) * 128]
                    if d == 0:
                        S.op("dve", lambda e, hv=hv, nv=nv, ds=ds: e.tensor_scalar(hv.ap, nv.ap[:, 0:128], ds[:, 5:6].ap, None, ALU.mult),
                             R=[nv, ds[:, :]], W=[hv])
                    else:
                        S.op("dve", lambda e, hv=hv, nv=nv, ds=ds: e.scalar_tensor_tensor(
                            hv.ap, nv.ap[:, 0:128], ds[:, 5:6].ap, hv.ap, ALU.mult, ALU.add),
                            R=[nv, ds[:, :], hv], W=[hv])
    for tt in range(8):
        ov = osg[tt % 2][:, :]
        S.dma("sp", ov, OS.v(OS.h.ap()[tt * 128:(tt + 1) * 128, :]), "osg%d" % (tt % 2))
        for hh in range(8):
            hv = hacc[:, tt, hh * 128:(hh + 1) * 128]
            S.op("dve", lambda e, hv=hv, hh=hh: e.bn_stats(gst2[:, hh, :].ap, hv.ap), R=[hv], W=[gst2[:, :, :]])
            S.op("dve", lambda e, hh=hh: e.bn_aggr(gag2[:, hh, :].ap, gst2[:, hh, :].ap), R=[gst2[:, :, :]], W=[gag2[:, :, :]])
        S.op("act", lambda e: e.activation(out=grs2[:, :].ap, in_=gag2[:, :, 1].ap, func=AF.Sqrt, bias=C.eps[:, :].ap),
             R=[gag2[:, :, :], C.eps[:, :]], W=[grs2[:, :]])
        S.op("dve", lambda e: e.reciprocal(out=grs2[:, :].ap, in_=grs2[:, :].ap), R=[grs2[:, :]], W=[grs2[:, :]])
        for hh in range(8):
            hv = hacc[:, tt, hh * 128:(hh + 1) * 128]
            S.op("dve", lambda e, hv=hv, hh=hh: e.tensor_scalar(
                yn2[:, hh * 128:(hh + 1) * 128].ap, hv.ap, gag2[:, hh, 0:1].ap, grs2[:, hh:hh + 1].ap, ALU.subtract, ALU.mult),
                R=[hv, gag2[:, :, :], grs2[:, :]], W=[yn2[:, :]])
        S.op("pool", lambda e: e.tensor_tensor(yn2[:, :].ap, yn2[:, :].ap, mnw[:, :].ap, ALU.mult), R=[yn2[:, :], mnw[:, :]], W=[yn2[:, :]])
        yv = ystg[tt % 2][:, :]
        S.op("dve", lambda e, yv=yv, ov=ov: e.tensor_tensor(yv.ap, yn2[:, :].ap, ov.ap, ALU.mult), R=[yn2[:, :], ov], W=[yv])
        S.dma("sp", C.ymix.v(C.ymix.h.ap()[tt * 128:(tt + 1) * 128, 1024:2048], 2, 4), yv, "ystg%d" % (tt % 2))


def phase_e(C, I, stage):
    nc, S, sb = C.nc, C.S, C.sb
    P0 = 8192
    K1 = 1024
    yacc = sb([128, 8, D], F32, P0)
    C.yacc = yacc
    Wout = sb([128, 16, D], BF16, P0 + 64 * K1)
    ytile = [sb([128, D], BF16, P0 + (128 + 4 * i) * K1) for i in range(2)]
    yT = [sb([128, 16, 128], BF16, P0 + (136 + 4 * i) * K1) for i in range(2)]
    r = sb([128, D], F32, P0 + 144 * K1)
    bout = sb([128, D], F32, P0 + 152 * K1)
    g1 = sb([128, D], F32, P0 + 160 * K1)
    b1 = sb([128, D], F32, P0 + 168 * K1)
    x1T = sb([128, 16, 128], F32, P0 + 176 * K1)
    x1st = sb([128, D], BF16, P0 + 184 * K1)
    Wr = sb([128, 16, NE], F32, P0 + 188 * K1)
    rb = sb([128, NE], F32, P0 + 190 * K1)
    lg = sb([128, NE], F32, P0 + 190 * K1 + 512)
    m8 = sb([128, 8], F32, P0 + 191 * K1)
    sm = sb([128, 8], F32, P0 + 191 * K1 + 512)
    ex = sb([128, NE], F32, P0 + 192 * K1)
    mk = sb([128, NE], F32, P0 + 192 * K1 + 512)
    lst = sb([128, 4, 6], F32, P0 + 193 * K1)
    lag_ = sb([128, 2], F32, P0 + 193 * K1 + 128)
    lrs = sb([128, 1], F32, P0 + 193 * K1 + 192)
    C.G = sb([128, 8, NE], F32, 6144)
    C.POS = sb([128, 8, NE], F32, 7168)
    C.x1s = C.dscr("x1s", [OWN, D], BF16, 8)
    WO = DBuf(I.w_out, "w_out")
    for q in range(4):
        S.dma("pool", Wout[:, q * 4:(q + 1) * 4, :],
              WO.v(I.w_out.ap()[q * 512:(q + 1) * 512, :].rearrange("(ct p) d -> p ct d", p=128)), "wout%d" % q)
    for nm, dst, h in (("boutb", bout, I.boutb), ("ln1g", g1, I.ln1g), ("ln1b", b1, I.ln1b), ("rbb", rb, I.rbb)):
        S.dma("sp", dst[:, :], DBuf(h, nm).v(h.ap()), "e_" + nm)
    S.dma("sp", Wr[:, :, :], DBuf(I.router_w, "router_w").v(I.router_w.ap().rearrange("(kt p) e -> p kt e", p=128)), "e_wr")
    X = DBuf(I.x, "x")
    YM = C.ymix
    for tt in range(8):
        yt = ytile[tt % 2]
        yTt = yT[tt % 2]
        S.dma("sp", yt[:, :], YM.v(YM.h.ap()[tt * 128:(tt + 1) * 128, :]), "ytile%d" % (tt % 2))
        ya = yacc[:, tt, :]
        S.dma("sp", ya, X.v(I.x.ap()[tt * 128:(tt + 1) * 128, :]), "xres")
        for hb in range(2):
            pi = nextps(C)
            pb = C.psb[pi]
            for j in range(8):
                ct = hb * 8 + j
                S.op("pe", lambda e, pb=pb, yt=yt, j=j, ct=ct: e.transpose(
                    pb[:, j * 128:(j + 1) * 128].ap, yt[:, ct * 128:(ct + 1) * 128].ap, C.idb[:, :].ap),
                    R=[yt[:, ct * 128:(ct + 1) * 128], C.idb[:, :]], W=[pb[:, j * 128:(j + 1) * 128]])
            dst = yTt[:, hb * 8:(hb + 1) * 8, :]
            S.op("act", lambda e, pb=pb, dst=dst: e.copy(out=dst.ap, in_=pb[:, :].ap.rearrange("p (a b) -> p a b", a=8)),
                 R=[pb[:, :]], W=[dst])
        for db in range(4):
            pi = nextps(C)
            pv = C.ps[pi][:, 0:512]

            def mm(e, pv=pv, yTt=yTt, db=db):
                for ct in range(16):
                    ins = e.matmul(pv.ap, yTt[:, ct, :].ap, Wout[:, ct, db * 512:(db + 1) * 512].ap, start=(ct == 0), stop=(ct == 15))
                return ins
            S.op("pe", mm, R=[yTt[:, :, :], Wout[:, :, db * 512:(db + 1) * 512]], W=[pv])
            rv = r[:, db * 512:(db + 1) * 512]
            S.op("dve", lambda e, pv=pv, rv=rv, db=db: e.tensor_tensor(rv.ap, pv.ap, bout[:, db * 512:(db + 1) * 512].ap, ALU.add),
                 R=[pv, bout[:, :]], W=[rv])
        S.op("dve", lambda e, ya=ya: e.scalar_tensor_tensor(r[:, :].ap, ya.ap, DN_ALPHA, r[:, :].ap, ALU.mult, ALU.add),
             R=[ya, r[:, :]], W=[r[:, :]])
        layer_norm_tile(C, r, lst, lag_, lrs, g1, b1)
        S.op("act", lambda e, ya=ya: e.mul(ya.ap, r[:, :].ap, DN_ALPHA), R=[r[:, :]], W=[ya])
        S.op("act", lambda e: e.copy(out=x1st[:, :].ap, in_=r[:, :].ap), R=[r[:, :]], W=[x1st[:, :]])
        S.dma("sp", C.x1s.v(C.x1s.h.ap()[tt * 128:(tt + 1) * 128, :], tt, tt + 1), x1st[:, :], "x1st")
        for qd in range(4):
            pi = nextps(C)
            pf = C.ps[pi]
            for j in range(4):
                kt = qd * 4 + j
                S.op("pe", lambda e, pf=pf, j=j, kt=kt: e.transpose(
                    pf[:, j * 128:(j + 1) * 128].ap, r[:, kt * 128:(kt + 1) * 128].ap, C.idf[:, :].ap),
                    R=[r[:, kt * 128:(kt + 1) * 128], C.idf[:, :]], W=[pf[:, j * 128:(j + 1) * 128]])
            dst = x1T[:, qd * 4:(qd + 1) * 4, :]
            S.op("dve", lambda e, pf=pf, dst=dst: e.tensor_copy(out=dst.ap, in_=pf[:, :].ap.rearrange("p (a b) -> p a b", a=4)),
                 R=[pf[:, :]], W=[dst])
        pi = nextps(C)
        pl = C.ps[pi][:, 0:NE]

        def mmr(e, pl=pl):
            for kt in range(16):
                ins = e.matmul(pl.ap, x1T[:, kt, :].ap, Wr[:, kt, :].ap, start=(kt == 0), stop=(kt == 15))
            return ins
        S.op("pe", mmr, R=[x1T[:, :, :], Wr[:, :, :]], W=[pl])
        S.op("dve", lambda e, pl=pl: e.tensor_tensor(lg[:, :].ap, pl.ap, rb[:, :].ap, ALU.add), R=[pl, rb[:, :]], W=[lg[:, :]])
        S.op("dve", lambda e: e.max(out=m8[:, :].ap, in_=lg[:, :].ap), R=[lg[:, :]], W=[m8[:, :]])
        S.op("dve", lambda e: e.tensor_scalar(mk[:, :].ap, lg[:, :].ap, m8[:, 3:4].ap, None, ALU.is_ge), R=[lg[:, :], m8[:, :]], W=[mk[:, :]])
        S.op("dve", lambda e: e.tensor_scalar(sm[:, 0:1].ap, m8[:, 0:1].ap, -1.0, None, ALU.mult), R=[m8[:, :]], W=[sm[:, :]])
        S.op("act", lambda e: e.activation(out=ex[:, :].ap, in_=lg[:, :].ap, func=AF.Exp, bias=sm[:, 0:1].ap), R=[lg[:, :], sm[:, :]], W=[ex[:, :]])
        S.op("dve", lambda e: e.tensor_tensor(ex[:, :].ap, ex[:, :].ap, mk[:, :].ap, ALU.mult), R=[ex[:, :], mk[:, :]], W=[ex[:, :]])
        S.op("dve", lambda e: e.reduce_sum(out=sm[:, 1:2].ap, in_=ex[:, :].ap, axis=AX.X), R=[ex[:, :]], W=[sm[:, :]])
        S.op("dve", lambda e: e.reciprocal(out=sm[:, 2:3].ap, in_=sm[:, 1:2].ap), R=[sm[:, :]], W=[sm[:, :]])
        gv = C.G[:, tt, :]
        S.op("dve", lambda e, gv=gv: e.tensor_scalar(gv.ap, ex[:, :].ap, sm[:, 2:3].ap, None, ALU.mult), R=[ex[:, :], sm[:, :]], W=[gv])


def layer_norm_tile(C, r, lst, lag_, lrs, g, b):
    S = C.S
    for j in range(4):
        S.op("dve", lambda e, j=j: e.bn_stats(lst[:, j, :].ap, r[:, j * 512:(j + 1) * 512].ap), R=[r[:, :]], W=[lst[:, :, :]])
    S.op("dve", lambda e: e.bn_aggr(lag_[:, :].ap, lst[:, :, :].ap.rearrange("p a b -> p (a b)")), R=[lst[:, :, :]], W=[lag_[:, :]])
    S.op("act", lambda e: e.activation(out=lrs[:, :].ap, in_=lag_[:, 1:2].ap, func=AF.Sqrt, bias=C.eps[:, :].ap),
         R=[lag_[:, :], C.eps[:, :]], W=[lrs[:, :]])
    S.op("dve", lambda e: e.reciprocal(out=lrs[:, :].ap, in_=lrs[:, :].ap), R=[lrs[:, :]], W=[lrs[:, :]])
    S.op("dve", lambda e: e.tensor_scalar(r[:, :].ap, r[:, :].ap, lag_[:, 0:1].ap, lrs[:, :].ap, ALU.subtract, ALU.mult),
         R=[r[:, :], lag_[:, :], lrs[:, :]], W=[r[:, :]])
    S.op("pool", lambda e: e.tensor_tensor(r[:, :].ap, r[:, :].ap, g[:, :].ap, ALU.mult), R=[r[:, :], g[:, :]], W=[r[:, :]])
    S.op("pool", lambda e: e.tensor_tensor(r[:, :].ap, r[:, :].ap, b[:, :].ap, ALU.add), R=[r[:, :], b[:, :]], W=[r[:, :]])


def phase_f(C, I, stage, ne):
    nc, S, sb = C.nc, C.S, C.sb
    P0 = 8192
    K1 = 1024
    yacc = C.yacc
    G, POS = C.G, C.POS
    x1bf = sb([128, 8, D], BF16, P0 + 64 * K1)
    NRING = 4
    Wring = [sb([128, 16, 512], BF16, P0 + (96 + 16 * i) * K1) for i in range(NRING)]
    Wring4 = [sb([128, 2, 16, 256], BF16, P0 + (96 + 16 * i) * K1) for i in range(NRING)]
    xgT = sb([128, 16, CAP], BF16, P0 + 160 * K1)
    actT = sb([128, 16, CAP], BF16, P0 + 168 * K1)
    sel = sb([128, 8, CAP], BF16, P0 + 176 * K1)
    selT = sb([128, 2, OWN], BF16, P0 + 180 * K1)
    oe = [sb([128, 2, 512], BF16, P0 + (184 + 2 * i) * K1) for i in range(2)]
    tg = [sb([128, CAP], F32, P0 + (188 + i) * K1) for i in range(2)]
    ts_ = [sb([128, CAP], F32, P0 + (190 + i) * K1) for i in range(2)]
    tu = [sb([128, CAP], F32, P0 + (192 + i) * K1) for i in range(2)]
    bgu = [sb([128, 32], F32, P0 + 194 * K1 + 128 * i) for i in range(2)]
    iota = sb([128, CAP], F32, P0 + 195 * K1)
    Mall = sb([128, 8, NE], F32, P0 + 196 * K1)
    cnt = sb([128, 8, NE], F32, P0 + 197 * K1)
    sut = sb([128, 128], F32, P0 + 198 * K1)
    GT = sb([32, OWN], F32, P0 + 96 * K1)
    bd = sb([32, D], F32, P0 + 100 * K1)
    S.dma("sp", iota[:, :], DBuf(I.iota, "iota").v(I.iota.ap()), "f_iota")
    S.dma("sp", sut[:, :], DBuf(I.sutri, "sutri").v(I.sutri.ap()), "f_sut")
    S.dma("sp", bd[:, :], DBuf(I.bdown, "bdown").v(I.bdown.ap()), "f_bd")
    S.dma("sp", x1bf[:, :, :], C.x1s.v(C.x1s.h.ap().rearrange("(tt p) d -> p tt d", p=128)), "f_x1bf")
    S.op("dve", lambda e: e.tensor_scalar(Mall[:, :, :].ap, G[:, :, :].ap, 0.0, None, ALU.is_gt), R=[G[:, :, :]], W=[Mall[:, :, :]])
    pi = nextps(C)
    pw = C.ps[pi][:, 0:256]
    S.op("pe", lambda e, pw=pw: e.matmul(pw.ap, sut[:, :].ap, Mall[:, :, :].ap.rearrange("p a b -> p (a b)"), start=True, stop=True),
         R=[sut[:, :], Mall[:, :, :]], W=[pw])
    pi = nextps(C)
    pc = C.ps[pi][:, 0:256]
    S.op("pe", lambda e, pc=pc: e.matmul(pc.ap, C.onesf[:, :].ap, Mall[:, :, :].ap.rearrange("p a b -> p (a b)"), start=True, stop=True),
         R=[C.onesf[:, :], Mall[:, :, :]], W=[pc])
    S.op("dve", lambda e, pc=pc: e.tensor_copy(out=cnt[:, :, :].ap.rearrange("p a b -> p (a b)"), in_=pc.ap), R=[pc], W=[cnt[:, :, :]])
    S.op("dve", lambda e, pw=pw: e.tensor_copy(out=POS[:, :, :].ap.rearrange("p a b -> p (a b)"), in_=pw.ap), R=[pw], W=[POS[:, :, :]])
    for tt in range(1, 8):
        pass
    S.op("dve", lambda e: e.tensor_copy(out=Mall[:, 0, :].ap, in_=cnt[:, 0, :].ap), R=[cnt[:, :, :]], W=[Mall[:, :, :]])
    for tt in range(1, 8):
        S.op("dve", lambda e, tt=tt: e.tensor_tensor(POS[:, tt, :].ap, POS[:, tt, :].ap, Mall[:, 0, :].ap, ALU.add),
             R=[POS[:, :, :], Mall[:, :, :]], W=[POS[:, :, :]])
        if tt < 7:
            S.op("dve", lambda e, tt=tt: e.tensor_tensor(Mall[:, 0, :].ap, Mall[:, 0, :].ap, cnt[:, tt, :].ap, ALU.add),
                 R=[cnt[:, :, :], Mall[:, :, :]], W=[Mall[:, :, :]])
    S.op("dve", lambda e: e.tensor_scalar(Mall[:, :, :].ap, G[:, :, :].ap, 0.0, None, ALU.is_gt), R=[G[:, :, :]], W=[Mall[:, :, :]])
    for tt in range(8):
        pi = nextps(C)
        pg = C.ps[pi][0:32, 0:128]
        S.op("pe", lambda e, pg=pg, tt=tt: e.transpose(pg.ap, G[:, tt, :].ap, C.idf[:, :].ap), R=[G[:, tt, :], C.idf[:, :]], W=[pg])
        S.op("act", lambda e, pg=pg, tt=tt: e.copy(out=GT[:, tt * 128:(tt + 1) * 128].ap, in_=pg.ap), R=[pg], W=[GT[:, tt * 128:(tt + 1) * 128]])
    for tt in range(8):
        for db in range(4):
            pi = nextps(C)
            pv = C.ps[pi][:, 0:512]
            S.op("pe", lambda e, pv=pv, tt=tt, db=db: e.matmul(pv.ap, GT[:, tt * 128:(tt + 1) * 128].ap, bd[:, db * 512:(db + 1) * 512].ap, start=True, stop=True),
                 R=[GT[:, :], bd[:, :]], W=[pv])
            yv = yacc[:, tt, db * 512:(db + 1) * 512]
            S.op("dve", lambda e, pv=pv, yv=yv: e.tensor_tensor(yv.ap, yv.ap, pv.ap, ALU.add), R=[pv, yv], W=[yv])
    WG = DBuf(I.w_gu, "w_gu")
    WDn = DBuf(I.w_down, "w_down")
    BG = DBuf(I.bgu, "bgu")
    gi = [0]
    di = [0]
    for ex_ in range(ne):
        bg = bgu[ex_ % 2]
        S.dma("sp", bg[:, :], BG.v(I.bgu.ap()[ex_]), "f_bgu%d" % (ex_ % 2))
        for tt in range(8):
            S.op("dve", lambda e, tt=tt, ex_=ex_: e.tensor_scalar(
                sel[:, tt, :].ap, iota[:, :].ap, POS[:, tt, ex_:ex_ + 1].ap, Mall[:, tt, ex_:ex_ + 1].ap, ALU.is_equal, ALU.mult),
                R=[iota[:, :], POS[:, tt, :], Mall[:, tt, :]], W=[sel[:, tt, :]])
        for kt in range(16):
            pi = nextps(C)
            pv = C.ps[pi][:, 0:CAP]

            def mmg(e, pv=pv, kt=kt):
                for tt in range(8):
                    ins = e.matmul(pv.ap, x1bf[:, tt, kt * 128:(kt + 1) * 128].ap, sel[:, tt, :].ap, start=(tt == 0), stop=(tt == 7))
                return ins
            S.op("pe", mmg, R=[x1bf[:, :, kt * 128:(kt + 1) * 128], sel[:, :, :]], W=[pv])
            if kt % 2 == 0:
                S.op("act", lambda e, pv=pv, kt=kt: e.copy(out=xgT[:, kt, :].ap, in_=pv.ap), R=[pv], W=[xgT[:, kt, :]])
            else:
                S.op("dve", lambda e, pv=pv, kt=kt: e.tensor_copy(out=xgT[:, kt, :].ap, in_=pv.ap), R=[pv], W=[xgT[:, kt, :]])
        for jt in range(2):
            pi = nextps(C)
            pb = C.psb[pi]
            for tt in range(8):
                S.op("pe", lambda e, pb=pb, tt=tt, jt=jt: e.transpose(
                    pb[:, tt * 128:(tt + 1) * 128].ap, sel[:, tt, jt * 128:(jt + 1) * 128].ap, C.idb[:, :].ap),
                    R=[sel[:, tt, :], C.idb[:, :]], W=[pb[:, tt * 128:(tt + 1) * 128]])
            S.op("act", lambda e, pb=pb, jt=jt: e.copy(out=selT[:, jt, :].ap, in_=pb[:, :].ap), R=[pb[:, :]], W=[selT[:, jt, :]])
        for fb in range(8):
            slot = gi[0] % NRING
            gi[0] += 1
            wg = Wring[slot]
            wg4 = Wring4[slot]
            S.dma("pool", wg4[:, 0, :, :], WG.v(I.w_gu.ap()[ex_, :, fb * 256:(fb + 1) * 256].rearrange("(kt p) f -> p kt f", p=128)),
                  "wgu%da" % slot)
            S.dma("pool", wg4[:, 1, :, :], WG.v(I.w_gu.ap()[ex_, :, 2048 + fb * 256:2048 + (fb + 1) * 256].rearrange("(kt p) f -> p kt f", p=128)),
                  "wgu%db" % slot)
            for fl in range(2):
                ft = fb * 2 + fl
                pig = nextps(C)
                pgv = C.ps[pig][:, 0:CAP]
                piu = nextps(C)
                puv = C.ps[piu][:, 0:CAP]
                for (pvv, half) in ((pgv, 0), (puv, 1)):
                    def mmu(e, pvv=pvv, half=half, wg4=wg4, fl=fl):
                        for kt in range(16):
                            ins = e.matmul(pvv.ap, wg4[:, half, kt, fl * 128:(fl + 1) * 128].ap, xgT[:, kt, :].ap,
                                           start=(kt == 0), stop=(kt == 15))
                        return ins
                    S.op("pe", mmu, R=[wg4[:, half, :, :], xgT[:, :, :]], W=[pvv])
                a = ft % 2
                S.op("dve", lambda e, pgv=pgv, a=a, bg=bg, ft=ft: e.tensor_scalar(
                    tg[a][:, :].ap, pgv.ap, bg[:, ft:ft + 1].ap, 7.0, ALU.add, ALU.min), R=[pgv, bg[:, :]], W=[tg[a][:, :]])
                S.op("act", lambda e, a=a: e.activation(out=ts_[a][:, :].ap, in_=tg[a][:, :].ap, func=AF.Sigmoid, scale=1.702),
                     R=[tg[a][:, :]], W=[ts_[a][:, :]])
                S.op("dve", lambda e, puv=puv, a=a, bg=bg, ft=ft: e.tensor_scalar(
                    tu[a][:, :].ap, puv.ap, bg[:, 16 + ft:17 + ft].ap, 7.0, ALU.add, ALU.min), R=[puv, bg[:, :]], W=[tu[a][:, :]])
                S.op("dve", lambda e, a=a: e.tensor_scalar(tu[a][:, :].ap, tu[a][:, :].ap, -7.0, 1.0, ALU.max, ALU.add),
                     R=[tu[a][:, :]], W=[tu[a][:, :]])
                S.op("dve", lambda e, a=a: e.tensor_tensor(tg[a][:, :].ap, tg[a][:, :].ap, ts_[a][:, :].ap, ALU.mult),
                     R=[tg[a][:, :], ts_[a][:, :]], W=[tg[a][:, :]])
                S.op("dve", lambda e, a=a, ft=ft: e.tensor_tensor(actT[:, ft, :].ap, tg[a][:, :].ap, tu[a][:, :].ap, ALU.mult),
                     R=[tg[a][:, :], tu[a][:, :]], W=[actT[:, ft, :]])
        def down_block(db):
            slot = gi[0] % NRING
            gi[0] += 1
            wd = Wring[slot]
            S.dma("pool", wd[:, :, :], WDn.v(I.w_down.ap()[ex_, :, db * 512:(db + 1) * 512].rearrange("(ft p) d -> p ft d", p=128)),
                  "wgu%da" % slot)
            oev = oe[db % 2]
            for jt in range(2):
                pi = nextps(C)
                pv = C.ps[pi][:, 0:512]

                def mmd(e, pv=pv, wd=wd, jt=jt):
                    for ft in range(16):
                        ins = e.matmul(pv.ap, actT[:, ft, jt * 128:(jt + 1) * 128].ap, wd[:, ft, :].ap, start=(ft == 0), stop=(ft == 15))
                    return ins
                S.op("pe", mmd, R=[actT[:, :, :], wd[:, :, :]], W=[pv])
                S.op("act", lambda e, pv=pv, oev=oev, jt=jt: e.copy(out=oev[:, jt, :].ap, in_=pv.ap), R=[pv], W=[oev[:, jt, :]])

        def scatter_block(db, ex_=ex_):
            oev = oe[db % 2]
            for tt in range(8):
                pi = nextps(C)
                pv = C.ps[pi][:, 0:512]

                def mms(e, pv=pv, oev=oev, tt=tt):
                    e.matmul(pv.ap, selT[:, 0, tt * 128:(tt + 1) * 128].ap, oev[:, 0, :].ap, start=True, stop=False)
                    return e.matmul(pv.ap, selT[:, 1, tt * 128:(tt + 1) * 128].ap, oev[:, 1, :].ap, start=False, stop=True)
                S.op("pe", mms, R=[selT[:, :, tt * 128:(tt + 1) * 128], oev[:, :, :]], W=[pv])
                yv = yacc[:, tt, db * 512:(db + 1) * 512]
                S.op("dve", lambda e, pv=pv, yv=yv, tt=tt, ex_=ex_: e.scalar_tensor_tensor(
                    yv.ap, pv.ap, G[:, tt, ex_:ex_ + 1].ap, yv.ap, ALU.mult, ALU.add), R=[pv, yv, G[:, tt, :]], W=[yv])

        down_block(0)
        for db in range(1, 4):
            down_block(db)
            scatter_block(db - 1)
        scatter_block(3)


def phase_g(C, I, stage):
    nc, S, sb = C.nc, C.S, C.sb
    P0 = 8192
    K1 = 1024
    yacc = C.yacc
    g2 = sb([128, D], F32, P0 + 64 * K1)
    b2 = sb([128, D], F32, P0 + 72 * K1)
    r = [sb([128, D], F32, P0 + (80 + 8 * i) * K1) for i in range(2)]
    lst = sb([128, 4, 6], F32, P0 + 96 * K1)
    lag_ = sb([128, 2], F32, P0 + 96 * K1 + 128)
    lrs = sb([128, 1], F32, P0 + 96 * K1 + 192)
    S.dma("sp", g2[:, :], DBuf(I.ln2g, "ln2g").v(I.ln2g.ap()), "g_g2")
    S.dma("sp", b2[:, :], DBuf(I.ln2b, "ln2b").v(I.ln2b.ap()), "g_b2")
    O = DBuf(I.out, "out", 8)
    for tt in range(8):
        rr = r[tt % 2]
        S.op("act", lambda e, rr=rr, tt=tt: e.copy(out=rr[:, :].ap, in_=yacc[:, tt, :].ap), R=[yacc[:, tt, :]], W=[rr[:, :]])
        layer_norm_tile(C, rr, lst, lag_, lrs, g2, b2)
        S.dma("sp", O.v(I.out.ap()[tt * 128:(tt + 1) * 128, :], tt, tt + 1), rr[:, :], "out%d" % (tt % 2))

def from_phases(C, stage, ne, dbg):
    I = ext_inputs(C, ne, stage)
    C.I = I
    phase_ab(C, I, stage)
    if stage >= 2 and "skipc" not in dbg:
        phase_c(C, I, stage)
    if stage >= 3:
        if "skipc" in dbg:
            C.ymix = C.dscr("ymix", [OWN, 2048], BF16, 4)
            C.eps = C.sb([128, 1], F32, 4128)
            C.onesf = C.sb([128, 128], F32, 4608)
            C.S.op("dve", lambda e: e.memset(C.eps[:, :].ap, LN_EPS), W=[C.eps[:, :]])
            C.S.op("dve", lambda e: e.memset(C.onesf[:, :].ap, 1.0), W=[C.onesf[:, :]])
        phase_d(C, I, stage)
    if stage >= 4:
        if "skipd" in dbg:
            C.ymix = getattr(C, "ymix", None) or C.dscr("ymix", [OWN, 2048], BF16, 4)
        phase_e(C, I, stage)
    if "G_o" in dbg:
        dump(C, "G_o", C.G[:, :, :], [128, 8, NE])
    if stage >= 5:
        phase_f(C, I, stage, ne)
        phase_g(C, I, stage)
    if "gates" in dbg:
        g = C.nc.dram_tensor("gates_o", [128, 16, 32], F32, kind="ExternalOutput")
        C.S.dma("sp", DBuf(g, "gates_o").v(g.ap()), C.gates[:, :, :], "dbg_g")


def prep_core_ab(inp, b, hf):
    rev = hf == 1
    x = inp["x"][b]
    if rev:
        x = x[::-1]
    w_in = inp["w_in"][0]
    b_in = inp["b_in"][0]
    if rev:
        gperm = np.concatenate([np.arange(7168), np.arange(7184, 7200), np.arange(7168, 7184)])
        w_in = w_in[:, gperm]
        b_in = b_in[gperm]
    hcw = inp["hy_conv_w"][0]
    mcw = inp["ml_conv_w"][0]
    if rev:
        hcw = hcw[::-1]
        mcw = mcw[::-1]
    cw = np.concatenate([hcw, mcw], axis=1)
    cb = np.concatenate([inp["hy_conv_b"][0], inp["ml_conv_b"][0]])
    convw = np.stack([cw[0], cw[1], cw[2], cb, b_in[:5120]], axis=-1)
    convw = convw.reshape(40, 128, 5).transpose(1, 0, 2)
    m = {
        "x": np.ascontiguousarray(x, dtype=np.float32),
        "w_in": np.ascontiguousarray(w_in, dtype=np.float32),
        "convw": np.ascontiguousarray(convw, dtype=np.float32),
        "bias_tok": np.ascontiguousarray(np.broadcast_to(b_in[5120:7200], (128, 2080)), dtype=np.float32),
        "idb": np.eye(128, dtype=np.float32).astype(ml_dtypes.bfloat16),
        "idf": np.eye(128, dtype=np.float32),
    }
    return m


_CONST_CACHE = {}


def hyena_consts():
    if "hy" in _CONST_CACHE:
        return _CONST_CACHE["hy"]
    t = np.linspace(0.0, 1.0, L, dtype=np.float64)
    bands = 16
    fb = np.linspace(1e-4, bands - 1, bands, dtype=np.float64)[None]
    w = 2.0 * np.pi * np.arange(L, dtype=np.float64)[:, None] / L
    z = np.concatenate([t[:, None], np.cos(fb * w), -np.sin(fb * w)], -1)
    zT = np.ascontiguousarray(z.T).astype(np.float32)
    deltas = np.abs(np.linspace(np.log(1e-2) / 1.5, np.log(1e-2) / 0.3, DH, dtype=np.float64))
    decay = np.exp(-t[:, None] * deltas[None]).astype(np.float32)
    n = np.arange(2048, dtype=np.float64)
    ang = 2.0 * np.pi * np.outer(n, n + 0.5) / 4096.0
    G = np.stack([np.cos(ang), np.sin(ang)], 0)
    tabA = G.reshape(2, 16, 128, 16, 128).transpose(3, 2, 0, 1, 4)
    tabB = G.reshape(2, 16, 128, 16, 128).transpose(1, 4, 0, 3, 2)
    tabA = np.ascontiguousarray(tabA).astype(np.float32).astype(ml_dtypes.bfloat16)
    tabB = np.ascontiguousarray(tabB).astype(np.float32).astype(ml_dtypes.bfloat16)
    _CONST_CACHE["hy"] = dict(zT=zT, decay=decay, tabA=tabA, tabB=tabB)
    return _CONST_CACHE["hy"]


def prep_core_c(inp, b, hf):
    rev = hf == 1
    m = dict(hyena_consts())
    w3 = inp["hy_filt_w3"][0]
    if rev:
        w3 = w3.reshape(64, 2, 2, DH)[:, :, ::-1, :].reshape(64, 4096)
    m["fw1"] = np.ascontiguousarray(inp["hy_filt_w1"][0], dtype=np.float32)
    m["fw2"] = np.ascontiguousarray(inp["hy_filt_w2"][0], dtype=np.float32)
    m["fw3"] = np.ascontiguousarray(w3, dtype=np.float32)
    fr = inp["hy_filt_freq"][0]
    m["fsm"] = np.ascontiguousarray(np.stack([inp["hy_filt_b1"][0], inp["hy_filt_b2"][0], fr[0], fr[1]], -1), dtype=np.float32)
    m["skipb"] = np.ascontiguousarray(np.broadcast_to(inp["hy_skip"][0][None], (128, 2, DH)), dtype=np.float32)
    m["hnw"] = np.ascontiguousarray(np.broadcast_to(inp["hy_norm_w"][0][None], (128, DH)), dtype=np.float32)
    lm = np.ones((128, 2), np.float32)
    lm[0, 0] = 0.0 if rev else 1.0
    lm[0, 1] = 1.0 if rev else 0.0
    m["lagmask"] = lm
    return m


def prep_core_d(inp, b, hf):
    p = np.arange(128)
    U = (p[:, None] <= p[None, :]).astype(np.float32)
    Lm = (p[:, None] >= p[None, :]).astype(np.float32)
    return {
        "tri": np.ascontiguousarray(np.stack([U, Lm], 1)),
        "mnw": np.ascontiguousarray(np.broadcast_to(inp["ml_norm_w"][0][None], (128, 1024)), dtype=np.float32),
    }


def prep_core_efg(inp, b, hf, ne=NE):
    bc = lambda a, n: np.ascontiguousarray(np.broadcast_to(np.asarray(a, np.float32)[None], (128, n)), dtype=np.float32)
    p = np.arange(128)
    m = {
        "w_out": np.ascontiguousarray(inp["w_out"][0], dtype=np.float32),
        "boutb": bc(inp["b_out"][0], D),
        "ln1g": bc(inp["ln1_g"][0], D),
        "ln1b": bc(inp["ln1_b"][0], D),
        "ln2g": bc(inp["ln2_g"][0], D),
        "ln2b": bc(inp["ln2_b"][0], D),
        "router_w": np.ascontiguousarray(inp["router_w"][0], dtype=np.float32),
        "rbb": bc(inp["router_b"][0], NE),
        "sutri": (p[:, None] < p[None, :]).astype(np.float32),
        "iota": np.ascontiguousarray(np.broadcast_to(np.arange(CAP, dtype=np.float32)[None], (128, CAP))),
        "bdown": np.ascontiguousarray(inp["b_down"][0], dtype=np.float32),
    }
    if "w_gu" in inp:
        m["w_gu"] = inp["w_gu"][0][:ne]
        m["w_down"] = inp["w_down"][0][:ne]
        m["bgu"] = np.ascontiguousarray(inp["b_gu"][0][:ne].reshape(ne, 32, 128).transpose(0, 2, 1), dtype=np.float32)
    return m


def prep_core(inp, b, hf, ne=NE):
    m = prep_core_ab(inp, b, hf)
    m.update(prep_core_c(inp, b, hf))
    m.update(prep_core_d(inp, b, hf))
    m.update(prep_core_efg(inp, b, hf, ne))
    return m


_NC_CACHE = {}


def kernel(**inputs):
    inp = {k: np.asarray(v) for k, v in inputs.items()}
    if "nc" not in _NC_CACHE:
        _NC_CACHE["nc"] = build(stage=99)
    nc = _NC_CACHE["nc"]
    maps = [prep_core(inp, c // 2, c % 2) for c in range(8)]
    res = run_bass_kernel_spmd(nc, maps, core_ids=list(range(8)))
    out = np.zeros((4, L, D), np.float32)
    for c in range(8):
        b, hf = c // 2, c % 2
        o = np.asarray(res.results[c]["out"], dtype=np.float32)
        if hf == 0:
            out[b, :OWN] = o
        else:
            out[b, OWN:] = o[::-1]
    return out
```

```python
import numpy as np
import ml_dtypes
from contextlib import ExitStack
import concourse.bass as bass
import concourse.mybir as mybir
from concourse.bass_utils import run_bass_kernel_spmd

F32 = mybir.dt.float32
BF16 = mybir.dt.bfloat16
ALU = mybir.AluOpType
AF = mybir.ActivationFunctionType
AX = mybir.AxisListType

CELL = 512
SB_BASE = 16896
SB_END = 229344


class View:
    __slots__ = ("ap", "space", "lo", "hi")

    def __init__(self, ap, space, lo, hi):
        self.ap, self.space, self.lo, self.hi = ap, space, lo, hi


class Buf:
    def __init__(self, h, shape, es, space, base):
        self.h, self.shape, self.es, self.space, self.base = h, list(shape), es, space, base
        st = [1] * len(shape)
        for i in range(len(shape) - 2, 0, -1):
            st[i] = st[i + 1] * shape[i + 1]
        self.st = st

    def __getitem__(self, idx):
        if not isinstance(idx, tuple):
            idx = (idx,)
        ap = self.h[idx]
        lo = 0
        hi = 0
        for d in range(1, len(self.shape)):
            n = self.shape[d]
            if d < len(idx):
                ix = idx[d]
                if isinstance(ix, slice):
                    a = 0 if ix.start is None else ix.start
                    b = n if ix.stop is None else ix.stop
                else:
                    a, b = ix, ix + 1
            else:
                a, b = 0, n
            lo += a * self.st[d]
            hi += (b - 1) * self.st[d]
        hi += 1
        blo = self.base + lo * self.es
        bhi = self.base + hi * self.es
        if self.space == "ps":
            return View(ap, self.space, self.base // 2048, self.base // 2048 + 1)
        return View(ap, self.space, blo // CELL, (bhi + CELL - 1) // CELL)


class DBuf:
    def __init__(self, h, name, nslots=1):
        self.h, self.name, self.n = h, name, nslots

    def v(self, ap, lo=0, hi=None):
        return View(ap, "dr:" + self.name, lo, self.n if hi is None else hi)


FREEVARS = {}


class Op:
    __slots__ = ("eng", "fn", "R", "W", "key", "deps", "signal", "sig", "idx")


class Sched:
    ENG = ("pe", "act", "dve", "pool", "sp")

    def __init__(self, nc):
        self.nc = nc
        self.ops = []
        self.psum_i = 0

    def op(self, eng, fn, R=(), W=(), key=None):
        o = Op()
        o.eng, o.fn, o.R, o.W, o.key = eng, fn, list(R), list(W), key
        o.deps, o.signal, o.sig, o.idx = None, False, None, len(self.ops)
        if hasattr(fn, "__code__"):
            for nm in fn.__code__.co_freevars:
                FREEVARS.setdefault(nm, set()).add(fn.__code__.co_firstlineno)
        self.ops.append(o)
        return o

    def dma(self, q, out, in_, key):
        return self.op(q, lambda e: e.dma_start(out=out.ap, in_=in_.ap), R=[in_], W=[out], key=key)

    def finalize(self, stack):
        nc = self.nc
        lastw = {}
        readers = {}
        for o in self.ops:
            deps = set()
            for v in o.R:
                for c in range(v.lo, v.hi):
                    k = (v.space, c)
                    w = lastw.get(k)
                    if w is not None:
                        deps.add(w)
            for v in o.W:
                for c in range(v.lo, v.hi):
                    k = (v.space, c)
                    w = lastw.get(k)
                    if w is not None:
                        deps.add(w)
                    r = readers.get(k)
                    if r:
                        deps.update(r)
            for v in o.R:
                for c in range(v.lo, v.hi):
                    readers.setdefault((v.space, c), set()).add(o.idx)
            for v in o.W:
                for c in range(v.lo, v.hi):
                    k = (v.space, c)
                    lastw[k] = o.idx
                    readers[k] = set()
            deps.discard(o.idx)
            latest = {}
            keep = []
            for d in deps:
                od = self.ops[d]
                if od.key is not None:
                    keep.append(d)
                else:
                    if od.eng == "pe" and o.eng == "pe" and o.key is None:
                        continue
                    if latest.get(od.eng, -1) < d:
                        latest[od.eng] = d
            keep.extend(latest.values())
            o.deps = keep
            for d in keep:
                self.ops[d].signal = True
        sems = {}

        def getsem(name):
            if name not in sems:
                sems[name] = stack.enter_context(nc.semaphore("s_" + name))
            return sems[name]

        cnt = {}
        for o in self.ops:
            if o.key is not None:
                k = "d_" + o.key
                cnt[k] = cnt.get(k, 0) + 16
                o.sig = (k, cnt[k])
            elif o.signal:
                k = "e_" + o.eng
                cnt[k] = cnt.get(k, 0) + 1
                o.sig = (k, cnt[k])
        for k in cnt:
            getsem(k)
        self.nsem = len(sems)
        engs = {"pe": nc.tensor, "act": nc.scalar, "dve": nc.vector, "pool": nc.gpsimd, "sp": nc.sync}
        seen = {e: {} for e in engs}
        for o in self.ops:
            e = engs[o.eng]
            need = {}
            sn = seen[o.eng]
            for d in o.deps:
                k, val = self.ops[d].sig
                if sn.get(k, 0) >= val:
                    continue
                if need.get(k, 0) < val:
                    need[k] = val
            for k, val in need.items():
                e.wait_ge(sems[k], val)
                sn[k] = val
            ins = o.fn(e)
            if o.sig is not None:
                ins.then_inc(sems[o.sig[0]], 16 if o.key is not None else 1)
        for k, val in cnt.items():
            if k.startswith("d_"):
                nc.sync.wait_ge(sems[k], val)


D = 2048
L = 2048
NT = L // 128
OWN = 1024
NOT_ = OWN // 128
DH = 1024
DIN = 7200
NE = 32
CAP = 256
LN_EPS = 1e-5
DN_ALPHA = 2.0 ** 0.25
PI = float(np.pi)


class Ctx:
    pass


def build(stage=99, ne=NE, dbg=()):
    nc = bass.Bass("TRN2", target_bir_lowering=False)
    S = Sched(nc)
    C = Ctx()
    C.nc, C.S = nc, S
    stack = ExitStack()
    C.stack = stack

    def din(name, shape, dt=F32):
        return nc.dram_tensor(name, list(shape), dt, kind="ExternalInput")

    def dscr(name, shape, dt, nslots=1):
        kind = "ExternalOutput" if name in dbg else "Internal"
        return DBuf(nc.dram_tensor(name, list(shape), dt, kind=kind), name, nslots)

    C.din, C.dscr = din, dscr
    C.sb_names = 0

    def sb(shape, dt, off):
        es = 4 if dt == F32 else 2
        n = 1
        for x in shape[1:]:
            n *= x
        assert off % 32 == 0 and off >= 0
        assert SB_BASE + off + n * es <= SB_END, ("sbuf overflow", shape, off)
        C.sb_names += 1
        h = nc.alloc_sbuf_tensor_at("t%d" % C.sb_names, list(shape), dt, offset=SB_BASE + off)
        return Buf(h, shape, es, "sb", SB_BASE + off)

    C.sb = sb
    C.ps = []
    C.psb = []
    for i in range(8):
        h = nc.alloc_psum_tensor("ps%d" % i, [128, 512], F32)
        C.ps.append(Buf(h, [128, 512], 4, "ps", i * 2048))
        C.psb.append(Buf(h.bitcast(BF16), [128, 1024], 2, "ps", i * 2048))
    C.psr = 0
    C.ps_hold = set()

    C.dbg = dbg
    try:
        from_phases(C, stage, ne, dbg)
    except StopBuild:
        pass
    S.finalize(stack)
    stack.close()
    return nc


def nextps(C):
    while True:
        C.psr = (C.psr + 1) % 8
        if C.psr not in C.ps_hold:
            return C.psr


def ext_inputs(C, ne, stage=99):
    din = C.din
    I = Ctx()
    I.x = din("x", [L, D])
    I.w_in = din("w_in", [D, DIN])
    I.convw = din("convw", [128, 40, 5])
    I.bias_tok = din("bias_tok", [128, 2080])
    I.idb = din("idb", [128, 128], BF16)
    I.idf = din("idf", [128, 128])
    I.zT = din("zT", [33, L])
    I.decay = din("decay", [L, DH])
    I.tabA = din("tabA", [16, 128, 2, 16, 128], BF16)
    I.tabB = din("tabB", [16, 128, 2, 16, 128], BF16)
    I.fw1 = din("fw1", [33, 64])
    I.fw2 = din("fw2", [64, 64])
    I.fw3 = din("fw3", [64, 4096])
    I.fsm = din("fsm", [64, 4])
    I.skipb = din("skipb", [128, 2, DH])
    I.hnw = din("hnw", [128, DH])
    I.lagmask = din("lagmask", [128, 2])
    I.tri = din("tri", [128, 2, 128])
    I.mnw = din("mnw", [128, 1024])
    if stage < 4:
        return I
    I.w_out = din("w_out", [2048, D])
    I.boutb = din("boutb", [128, D])
    I.ln1g = din("ln1g", [128, D])
    I.ln1b = din("ln1b", [128, D])
    I.ln2g = din("ln2g", [128, D])
    I.ln2b = din("ln2b", [128, D])
    I.router_w = din("router_w", [D, NE])
    I.rbb = din("rbb", [128, NE])
    I.sutri = din("sutri", [128, 128])
    I.iota = din("iota", [128, CAP])
    if stage < 5:
        return I
    I.w_gu = din("w_gu", [ne, D, 4096])
    I.w_down = din("w_down", [ne, 2048, D])
    I.bgu = din("bgu", [ne, 128, 32])
    I.bdown = din("bdown", [NE, D])
    I.out = C.nc.dram_tensor("out", [OWN, D], F32, kind="ExternalOutput")
    return I


def phase_ab(C, I, stage):
    nc, S, sb = C.nc, C.S, C.sb
    C.idb = sb([128, 128], BF16, 0)
    C.idf = sb([128, 128], F32, 512)
    C.convw = sb([128, 40, 5], F32, 1024)
    C.gates = sb([128, 16, 32], F32, 2048)
    S.dma("sp", C.idb[:, :], DBuf(I.idb, "idb").v(I.idb.ap()), "c0")
    S.dma("sp", C.idf[:, :], DBuf(I.idf, "idf").v(I.idf.ap()), "c1")
    S.dma("sp", C.convw[:, :, :], DBuf(I.convw, "convw").v(I.convw.ap()), "c2")
    C.hyu = C.dscr("hyu", [3, L, DH], BF16, 6)
    C.qT = C.dscr("qT", [1024, L], BF16, 8)
    C.kT = C.dscr("kT", [1024, L], BF16, 8)
    C.ktok = C.dscr("ktok", [L, 1024], BF16, 2)
    C.vtok = C.dscr("vtok", [L, 1024], BF16, 2)
    C.osig = C.dscr("osig", [OWN, 1024], F32, 2)
    P0 = 8192
    xT = sb([128, 16, L], BF16, P0)
    wblk = [sb([128, 16, 512], BF16, P0 + 65536 + i * 16384) for i in range(2)]
    Q = P0 + 65536 + 32768
    xb = [sb([128, D], BF16, Q + i * 4096) for i in range(2)]
    u = [sb([128, L], F32, Q + i * 8192) for i in range(2)]
    cv = [sb([128, L], F32, Q + 16384 + i * 8192) for i in range(2)]
    cvb = [sb([128, L], BF16, Q + 32768 + i * 4096) for i in range(4)]
    stg = sb([128, 16, 512], BF16, Q + 49152)
    stgf = sb([128, 8, 512], F32, Q + 65536)
    btok = sb([128, 2080], F32, Q + 81920)
    tmpf = [sb([128, 512], F32, Q + 90624 + i * 2048) for i in range(2)]
    X = DBuf(I.x, "x")
    W = DBuf(I.w_in, "w_in")
    S.dma("sp", btok[:, :], DBuf(I.bias_tok, "bias_tok").v(I.bias_tok.ap()), "c3")
    for tt in range(NT):
        b = xb[tt % 2]
        S.dma("pool", b[:, :], X.v(I.x.ap()[tt * 128:(tt + 1) * 128, :]), "xb%d" % (tt % 2))
        for hb in range(2):
            pi = nextps(C)
            pb = C.psb[pi]
            for j in range(8):
                dt_ = hb * 8 + j
                S.op("pe", lambda e, pb=pb, b=b, j=j, dt_=dt_: e.transpose(
                    pb[:, j * 128:(j + 1) * 128].ap, b[:, dt_ * 128:(dt_ + 1) * 128].ap, C.idb[:, :].ap),
                    R=[b[:, dt_ * 128:(dt_ + 1) * 128], C.idb[:, :]], W=[pb[:, j * 128:(j + 1) * 128]])
            src = pb[:, :]
            dst = xT[:, hb * 8:(hb + 1) * 8, tt * 128:(tt + 1) * 128]
            eng = "dve" if hb == 0 else "act"
            if eng == "dve":
                S.op("dve", lambda e, src=src, dst=dst: e.tensor_copy(
                    out=dst.ap, in_=src.ap.rearrange("p (a b) -> p a b", a=8)), R=[src], W=[dst])
            else:
                S.op("act", lambda e, src=src, dst=dst: e.copy(
                    out=dst.ap, in_=src.ap.rearrange("p (a b) -> p a b", a=8)), R=[src], W=[dst])

    def load_w(c0, n, slot):
        wv = wblk[slot][:, :, 0:n]
        S.dma("pool", wv, W.v(I.w_in.ap()[:, c0:c0 + n].rearrange("(kt p) c -> p kt c", p=128)),
              "wblk%d" % slot)

    blk = 0
    for jb in range(10):
        slot = blk % 2
        blk += 1
        load_w(jb * 512, 512, slot)
        wb = wblk[slot]
        for m in range(4):
            ct = jb * 4 + m
            uu = u[ct % 2]
            cc = cv[ct % 2]
            for tb in range(4):
                pi = nextps(C)
                pv = C.ps[pi][:, 0:512]

                def mm(e, pv=pv, wb=wb, m=m, tb=tb):
                    for kt in range(16):
                        ins = e.matmul(pv.ap, wb[:, kt, m * 128:(m + 1) * 128].ap,
                                       xT[:, kt, tb * 512:(tb + 1) * 512].ap, start=(kt == 0), stop=(kt == 15))
                    return ins
                S.op("pe", mm, R=[wb[:, :, :], xT[:, :, tb * 512:(tb + 1) * 512]], W=[pv])
                uv = uu[:, tb * 512:(tb + 1) * 512]
                S.op("act", lambda e, pv=pv, uv=uv, ct=ct: e.activation(
                    out=uv.ap, in_=pv.ap, func=AF.Identity, bias=C.convw[:, ct, 4:5].ap),
                    R=[pv, C.convw[:, :, :]], W=[uv])
            S.op("dve", lambda e, uu=uu, cc=cc, ct=ct: e.tensor_scalar(
                cc[:, :].ap, uu[:, :].ap, C.convw[:, ct, 1:2].ap, C.convw[:, ct, 3:4].ap, ALU.mult, ALU.add),
                R=[uu[:, :], C.convw[:, :, :]], W=[cc[:, :]])
            S.op("dve", lambda e, uu=uu, cc=cc, ct=ct: e.scalar_tensor_tensor(
                cc[:, 1:L].ap, uu[:, 0:L - 1].ap, C.convw[:, ct, 0:1].ap, cc[:, 1:L].ap, ALU.mult, ALU.add),
                R=[uu[:, :], cc[:, :], C.convw[:, :, :]], W=[cc[:, :]])
            S.op("dve", lambda e, uu=uu, cc=cc, ct=ct: e.scalar_tensor_tensor(
                cc[:, 0:L - 1].ap, uu[:, 1:L].ap, C.convw[:, ct, 2:3].ap, cc[:, 0:L - 1].ap, ALU.mult, ALU.add),
                R=[uu[:, :], cc[:, :], C.convw[:, :, :]], W=[cc[:, :]])
            cb_ = cvb[m]
            if jb < 6:
                S.op("act", lambda e, cc=cc, cb_=cb_: e.copy(out=cb_[:, :].ap, in_=cc[:, :].ap),
                     R=[cc[:, :]], W=[cb_[:, :]])
            elif jb < 8:
                S.op("act", lambda e, cc=cc, cb_=cb_: e.activation(out=cb_[:, :].ap, in_=cc[:, :].ap, func=AF.Silu),
                     R=[cc[:, :]], W=[cb_[:, :]])
                hh = (jb - 6) * 4 + m
                S.dma("sp", C.qT.v(C.qT.h.ap()[hh * 128:(hh + 1) * 128, :], hh, hh + 1), cb_[:, :], "qT%d" % m)
            else:
                S.op("act", lambda e, cc=cc: e.activation(out=cc[:, :].ap, in_=cc[:, :].ap, func=AF.Silu),
                     R=[cc[:, :]], W=[cc[:, :]])
                S.op("dve", lambda e, cc=cc, cb_=cb_: e.tensor_scalar(
                    cb_[:, :].ap, cc[:, :].ap, float(128 ** -0.5), None, ALU.mult),
                    R=[cc[:, :]], W=[cb_[:, :]])
                hh = (jb - 8) * 4 + m
                S.dma("sp", C.kT.v(C.kT.h.ap()[hh * 128:(hh + 1) * 128, :], hh, hh + 1), cb_[:, :], "kT%d" % m)
        if jb < 6 or jb >= 8:
            for tt in range(NT):
                pi = nextps(C)
                pb = C.psb[pi]
                for m in range(4):
                    S.op("pe", lambda e, pb=pb, m=m, tt=tt: e.transpose(
                        pb[:, m * 128:(m + 1) * 128].ap, cvb[m][:, tt * 128:(tt + 1) * 128].ap, C.idb[:, :].ap),
                        R=[cvb[m][:, tt * 128:(tt + 1) * 128], C.idb[:, :]], W=[pb[:, m * 128:(m + 1) * 128]])
                sv = stg[:, tt, :]
                if tt % 2 == 0:
                    S.op("dve", lambda e, pb=pb, sv=sv: e.tensor_copy(out=sv.ap, in_=pb[:, 0:512].ap),
                         R=[pb[:, 0:512]], W=[sv])
                else:
                    S.op("act", lambda e, pb=pb, sv=sv: e.copy(out=sv.ap, in_=pb[:, 0:512].ap),
                         R=[pb[:, 0:512]], W=[sv])
            if jb < 6:
                w_, cbk = jb // 2, jb % 2
                dst = C.hyu.v(C.hyu.h.ap()[w_, :, cbk * 512:(cbk + 1) * 512].rearrange("(tt p) c -> p tt c", p=128),
                              jb, jb + 1)
            else:
                cbk = jb - 8
                dst = C.ktok.v(C.ktok.h.ap()[:, cbk * 512:(cbk + 1) * 512].rearrange("(tt p) c -> p tt c", p=128),
                               cbk, cbk + 1)
            S.dma("sp", dst, stg[:, :, :], "stg")
    for jb in range(5):
        slot = blk % 2
        blk += 1
        c0 = 5120 + jb * 512
        n = 512 if jb < 4 else 32
        load_w(c0, n, slot)
        wb = wblk[slot]
        ntt = NT if (jb < 2 or jb == 4) else NOT_
        for tt in range(ntt):
            pi = nextps(C)
            pv = C.ps[pi][:, 0:n]

            def mm(e, pv=pv, wb=wb, tt=tt, n=n):
                for kt in range(16):
                    ins = e.matmul(pv.ap, xT[:, kt, tt * 128:(tt + 1) * 128].ap, wb[:, kt, 0:n].ap,
                                   start=(kt == 0), stop=(kt == 15))
                return ins
            S.op("pe", mm, R=[wb[:, :, :], xT[:, :, tt * 128:(tt + 1) * 128]], W=[pv])
            bv = btok[:, c0 - 5120:c0 - 5120 + n]
            if jb < 2:
                sv = stg[:, tt, :]
                S.op("dve", lambda e, pv=pv, sv=sv, bv=bv: e.tensor_tensor(sv.ap, pv.ap, bv.ap, ALU.add),
                     R=[pv, bv], W=[sv])
            elif jb < 4:
                tv = tmpf[tt % 2][:, :]
                S.op("dve", lambda e, pv=pv, tv=tv, bv=bv: e.tensor_tensor(tv.ap, pv.ap, bv.ap, ALU.add),
                     R=[pv, bv], W=[tv])
                sv = stgf[:, tt, :]
                S.op("act", lambda e, tv=tv, sv=sv: e.activation(out=sv.ap, in_=tv.ap, func=AF.Sigmoid),
                     R=[tv], W=[sv])
            else:
                gv = C.gates[:, tt, :]
                S.op("dve", lambda e, pv=pv, gv=gv, bv=bv: e.tensor_tensor(gv.ap, pv.ap, bv.ap, ALU.add),
                     R=[pv, bv], W=[gv])
        if jb < 2:
            dst = C.vtok.v(C.vtok.h.ap()[:, jb * 512:(jb + 1) * 512].rearrange("(tt p) c -> p tt c", p=128), jb, jb + 1)
            S.dma("sp", dst, stg[:, :, :], "stg")
        elif jb < 4:
            cbk = jb - 2
            dst = C.osig.v(C.osig.h.ap()[:, cbk * 512:(cbk + 1) * 512].rearrange("(tt p) c -> p tt c", p=128), cbk, cbk + 1)
            S.dma("sp", dst, stgf[:, :, :], "stgf")


def dump(C, name, view, shape, dt=F32):
    h = C.nc.dram_tensor(name, list(shape), dt, kind="ExternalOutput")
    C.S.dma("sp", DBuf(h, name).v(h.ap()), view, "dump_" + name)


class StopBuild(Exception):
    pass


def act_sin(C, out, arg, tmp1, tmp2, R, W):
    S = C.S
    S.op("act", lambda e: e.activation(out=tmp1.ap, in_=arg.ap, func=AF.Sin, scale=0.5), R=[arg], W=[tmp1])
    S.op("act", lambda e: e.activation(out=tmp2.ap, in_=arg.ap, func=AF.Abs), R=[arg], W=[tmp2])
    S.op("act", lambda e: e.activation(out=tmp2.ap, in_=tmp2.ap, func=AF.Sin, scale=-0.5, bias=C.hpi[:, :].ap[0:64]),
         R=[tmp2, C.hpi[:, :]], W=[tmp2])
    S.op("dve", lambda e: e.scalar_tensor_tensor(out.ap, tmp1.ap, 2.0, tmp2.ap, ALU.mult, ALU.mult),
         R=[tmp1, tmp2], W=[out])


def phase_c(C, I, stage):
    nc, S, sb = C.nc, C.S, C.sb
    P0 = 8192
    K1 = 1024
    C.ymix = C.dscr("ymix", [OWN, 2048], BF16, 4)
    C.hpi = sb([128, 1], F32, 4096)
    C.eps = sb([128, 1], F32, 4128)
    C.onesf = sb([128, 128], F32, 4608)
    C.onesb = sb([128, 128], BF16, 4224)
    S.op("dve", lambda e: e.memset(C.onesb[:, :].ap, 1.0), W=[C.onesb[:, :]])
    lagm = sb([128, 2], F32, 4160)
    S.op("dve", lambda e: e.memset(C.hpi[:, :].ap, PI / 2), W=[C.hpi[:, :]])
    S.op("dve", lambda e: e.memset(C.eps[:, :].ap, LN_EPS), W=[C.eps[:, :]])
    S.op("dve", lambda e: e.memset(C.onesf[:, :].ap, 1.0), W=[C.onesf[:, :]])
    S.dma("sp", lagm[:, :], DBuf(I.lagmask, "lagmask").v(I.lagmask.ap()), "c4")
    zT = sb([33, L], F32, P0)
    h2T = sb([64, L], F32, P0 + 8 * K1)
    h1T = sb([64, L], F32, P0 + 16 * K1)
    w3 = sb([64, 4096], F32, P0 + 24 * K1)
    w1 = sb([33, 64], F32, P0 + 40 * K1)
    w2 = sb([64, 64], F32, P0 + 40 * K1 + 256)
    fsm = sb([64, 4], F32, P0 + 40 * K1 + 512)
    skipb = sb([128, 2, DH], F32, P0 + 41 * K1)
    hnw = sb([128, DH], F32, P0 + 49 * K1)
    vz = sb([128, 16, 512], BF16, P0 + 54 * K1)
    Kre = sb([128, 16, 512], BF16, P0 + 70 * K1)
    Kim = sb([128, 16, 512], BF16, P0 + 86 * K1)
    tabs = [sb([128, 2, 16, 128], BF16, P0 + (102 + 8 * i) * K1) for i in range(3)]
    Pb = sb([128, 16, 512], BF16, P0 + 126 * K1)
    Qb = sb([128, 16, 512], BF16, P0 + 142 * K1)
    kp, km = Pb, Qb
    XR = [sb([128, 512], F32, P0 + (158 + 2 * i) * K1) for i in range(2)]
    XS = [sb([128, 512], F32, P0 + (162 + 2 * i) * K1) for i in range(2)]
    T = [sb([128, 512], F32, P0 + (166 + 2 * i) * K1) for i in range(4)]
    zz = sb([128, 8, 512], F32, P0 + 158 * K1)
    x1t = [sb([128, 512], BF16, P0 + (174 + i) * K1) for i in range(3)]
    ystage = sb([128, 8, 512], BF16, P0 + 177 * K1)
    dec = [sb([128, 512], F32, P0 + 2 * i * K1) for i in range(2)]
    kft2 = [[sb([128, 512], F32, P0 + (18 + 2 * i) * K1) for i in range(2)],
            [sb([128, 512], F32, P0 + (4 + 2 * i) * K1) for i in range(2)]]
    absb2 = [[sb([128, 512], BF16, P0 + (168 + 2 * s_) * K1 + 1024 * i) for i in range(2)] for s_ in range(2)]
    sc = sb([128, 512], F32, P0 + 16 * K1)
    kft = [sb([128, 512], F32, P0 + (18 + 2 * i) * K1) for i in range(2)]
    gst = sb([128, 4, 6], F32, P0 + 185 * K1)
    gag = sb([128, 4, 2], F32, P0 + 185 * K1 + 128)
    grs = sb([128, 4], F32, P0 + 185 * K1 + 192)
    yn = sb([128, 512], F32, P0 + 186 * K1)

    def ld(dst, h, name, key, q="sp"):
        S.dma(q, dst, DBuf(h, name).v(h.ap()), key)
    ld(zT[:, :], I.zT, "zT", "c5")
    ld(w1[:, :], I.fw1, "fw1", "c6")
    ld(w2[:, :], I.fw2, "fw2", "c7")
    ld(w3[:, :], I.fw3, "fw3", "c8")
    ld(fsm[:, :], I.fsm, "fsm", "c9")
    ld(skipb[:, :, :], I.skipb, "skipb", "c10")
    ld(hnw[:, :], I.hnw, "hnw", "c11")
    S.op("dve", lambda e: e.tensor_scalar(skipb[:, :, :].ap, skipb[:, :, :].ap, 1.0 / 2048.0, None, ALU.mult),
         R=[skipb[:, :, :]], W=[skipb[:, :, :]])
    for layer in range(2):
        src = zT if layer == 0 else h1T
        dstT = h1T if layer == 0 else h2T
        ww = w1 if layer == 0 else w2
        for tb in range(4):
            pi = nextps(C)
            pv = C.ps[pi][0:64, 0:512]
            sv = src[:, tb * 512:(tb + 1) * 512]
            S.op("pe", lambda e, pv=pv, ww=ww, sv=sv: e.matmul(pv.ap, ww[:, :].ap, sv.ap, start=True, stop=True),
                 R=[ww[:, :], sv], W=[pv])
            a_ = T[0][0:64, :]
            S.op("dve", lambda e, pv=pv, a_=a_, layer=layer: e.tensor_scalar(
                a_.ap, pv.ap, fsm[:, layer:layer + 1].ap, fsm[:, 2 + layer:3 + layer].ap, ALU.add, ALU.mult),
                R=[pv, fsm[:, :]], W=[a_])
            act_sin(C, dstT[:, tb * 512:(tb + 1) * 512], a_, T[1][0:64, :], T[2][0:64, :], None, None)

    if "c1" in C.dbg:
        dump(C, "h2T_o", h2T[:, :], [64, L])
        dump(C, "h1T_o", h1T[:, :], [64, L])
        raise StopBuild()
    TA = DBuf(I.tabA, "tabA")
    TB = DBuf(I.tabB, "tabB")
    DEC = DBuf(I.decay, "decay")
    HY = C.hyu
    tabi = [0]

    def load_tab(Tb, h, idx):
        slot = tabi[0] % 3
        tabi[0] += 1
        S.dma("sp", tabs[slot][:, :, :, :], Tb.v(h.ap()[idx]), "tab%d" % slot)
        return tabs[slot]

    def fwd_dft(ft, src):
        tb_ = load_tab(TA, I.tabA, ft)
        pis = []
        for cs in range(2):
            pi = nextps(C)
            pis.append(pi)
            pv = C.ps[pi][:, 0:512]

            def mm(e, pv=pv, tb_=tb_, cs=cs):
                for st in range(16):
                    ins = e.matmul(pv.ap, tb_[:, cs, st, :].ap, src[:, st, :].ap, start=(st == 0), stop=(st == 15))
                return ins
            S.op("pe", mm, R=[tb_[:, cs, :, :], src[:, :, :]], W=[pv])
        return pis

    for cb in range(2):
        cs0 = cb * 512
        for o in range(2):
            hold = nextps(C)
            C.ps_hold.add(hold)
            Sps = C.ps[hold][:, 0:512]
            def taps_stage1(lt, o=o, cs0=cs0):
                st_ = lt % 2
                dv = dec[st_][:, :]
                S.dma("sp", dv, DEC.v(I.decay.ap()[lt * 128:(lt + 1) * 128, cs0:cs0 + 512]), "dec%d" % st_)
                kk = []
                for d_ in range(2):
                    pi = nextps(C)
                    pv = C.ps[pi][:, 0:512]
                    col = o * 2048 + d_ * 1024 + cs0
                    S.op("pe", lambda e, pv=pv, lt=lt, col=col: e.matmul(
                        pv.ap, h2T[:, lt * 128:(lt + 1) * 128].ap, w3[:, col:col + 512].ap, start=True, stop=True),
                        R=[h2T[:, :], w3[:, :]], W=[pv])
                    kv = kft2[st_][d_][:, :]
                    S.op("dve", lambda e, pv=pv, kv=kv, dv=dv: e.tensor_tensor(kv.ap, pv.ap, dv.ap, ALU.mult),
                         R=[pv, dv], W=[kv])
                    if lt == 0:
                        S.op("dve", lambda e, kv=kv, d_=d_: e.tensor_scalar(
                            kv.ap, kv.ap, lagm[:, d_:d_ + 1].ap, None, ALU.mult), R=[kv, lagm[:, :]], W=[kv])
                    av = absb2[st_][d_][:, :]
                    S.op("act", lambda e, kv=kv, av=av: e.activation(out=av.ap, in_=kv.ap, func=AF.Abs), R=[kv], W=[av])
                    kk.append(kv)
                S.op("dve", lambda e, lt=lt, kk=kk: e.tensor_tensor(kp[:, lt, :].ap, kk[0].ap, kk[1].ap, ALU.add),
                     R=[kk[0], kk[1]], W=[kp[:, lt, :]])
                S.op("dve", lambda e, lt=lt, kk=kk: e.tensor_tensor(km[:, lt, :].ap, kk[1].ap, kk[0].ap, ALU.subtract),
                     R=[kk[0], kk[1]], W=[km[:, lt, :]])

            def taps_stage2(lt, Sps=Sps):
                st_ = lt % 2
                for d_ in range(2):
                    av = absb2[st_][d_][:, :]
                    S.op("pe", lambda e, av=av, lt=lt, d_=d_, Sps=Sps: e.matmul(
                        Sps.ap, C.onesb[:, :].ap, av.ap, start=(lt == 0 and d_ == 0), stop=(lt == 15 and d_ == 1)),
                        R=[av, C.onesb[:, :]], W=[Sps])

            taps_stage1(0)
            for lt in range(1, 16):
                taps_stage1(lt)
                taps_stage2(lt - 1)
            taps_stage2(15)
            S.op("dve", lambda e, Sps=Sps: e.tensor_scalar(sc[:, :].ap, Sps.ap, 2048.0, None, ALU.mult), R=[Sps], W=[sc[:, :]])
            S.op("dve", lambda e: e.reciprocal(out=sc[:, :].ap, in_=sc[:, :].ap), R=[sc[:, :]], W=[sc[:, :]])
            C.ps_hold.discard(hold)
            for ft in range(16):
                tb_ = load_tab(TA, I.tabA, ft)
                for cs in range(2):
                    pi = nextps(C)
                    pv = C.ps[pi][:, 0:512]
                    srck = kp if cs == 0 else km

                    def mm(e, pv=pv, tb_=tb_, cs=cs, srck=srck):
                        for st in range(16):
                            ins = e.matmul(pv.ap, tb_[:, cs, st, :].ap, srck[:, st, :].ap, start=(st == 0), stop=(st == 15))
                        return ins
                    S.op("pe", mm, R=[tb_[:, cs, :, :], srck[:, :, :]], W=[pv])
                    if cs == 0:
                        tv = T[0][:, :]
                        S.op("dve", lambda e, pv=pv, tv=tv: e.tensor_tensor(tv.ap, pv.ap, sc[:, :].ap, ALU.mult),
                             R=[pv, sc[:, :]], W=[tv])
                        S.op("pool", lambda e, tv=tv, ft=ft, o=o, cs0=cs0: e.tensor_tensor(
                            Kre[:, ft, :].ap, tv.ap, skipb[:, o, cs0:cs0 + 512].ap, ALU.add),
                            R=[tv, skipb[:, :, :]], W=[Kre[:, ft, :]])
                    else:
                        S.op("dve", lambda e, pv=pv, ft=ft: e.tensor_tensor(Kim[:, ft, :].ap, pv.ap, sc[:, :].ap, ALU.mult),
                             R=[pv, sc[:, :]], W=[Kim[:, ft, :]])
            if "spec%d" % o in C.dbg:
                dump(C, "Kre_o", Kre[:, :, :], [128, 16, 512], BF16)
                raise StopBuild()
            if "c2" in C.dbg:
                dump(C, "kp_o", kp[:, :, :], [128, 16, 512], BF16)
                dump(C, "km_o", km[:, :, :], [128, 16, 512], BF16)
                dump(C, "Kre_o", Kre[:, :, :], [128, 16, 512], BF16)
                dump(C, "Kim_o", Kim[:, :, :], [128, 16, 512], BF16)
                dump(C, "sc_o", sc[:, :], [128, 512])
                raise StopBuild()
            if o == 0:
                S.dma("sp", vz[:, :, :], HY.v(HY.h.ap()[0, :, cs0:cs0 + 512].rearrange("(tt p) c -> p tt c", p=128),
                                             cb, cb + 1), "vz")
            for ft in range(16):
                pr, ps_ = fwd_dft(ft, vz)
                xr = XR[ft % 2][:, :]
                xs = XS[ft % 2][:, :]
                S.op("act", lambda e, pr=pr, xr=xr: e.copy(out=xr.ap, in_=C.ps[pr][:, 0:512].ap), R=[C.ps[pr][:, 0:512]], W=[xr])
                S.op("act", lambda e, ps_=ps_, xs=xs: e.copy(out=xs.ap, in_=C.ps[ps_][:, 0:512].ap), R=[C.ps[ps_][:, 0:512]], W=[xs])
                kr = Kre[:, ft, :]
                ki = Kim[:, ft, :]
                S.op("dve", lambda e, xr=xr, kr=kr: e.tensor_tensor(T[0][:, :].ap, xr.ap, kr.ap, ALU.mult), R=[xr, kr], W=[T[0][:, :]])
                S.op("dve", lambda e, xs=xs, ki=ki: e.tensor_tensor(T[1][:, :].ap, xs.ap, ki.ap, ALU.mult), R=[xs, ki], W=[T[1][:, :]])
                S.op("dve", lambda e, ft=ft: e.tensor_tensor(Pb[:, ft, :].ap, T[0][:, :].ap, T[1][:, :].ap, ALU.add),
                     R=[T[0][:, :], T[1][:, :]], W=[Pb[:, ft, :]])
                S.op("dve", lambda e, xs=xs, kr=kr: e.tensor_tensor(T[2][:, :].ap, xs.ap, kr.ap, ALU.mult), R=[xs, kr], W=[T[2][:, :]])
                S.op("pool", lambda e, xr=xr, ki=ki: e.tensor_tensor(T[3][:, :].ap, xr.ap, ki.ap, ALU.mult), R=[xr, ki], W=[T[3][:, :]])
                S.op("pool", lambda e, ft=ft: e.tensor_tensor(Qb[:, ft, :].ap, T[2][:, :].ap, T[3][:, :].ap, ALU.subtract),
                     R=[T[2][:, :], T[3][:, :]], W=[Qb[:, ft, :]])
            if "fwd%d" % o in C.dbg:
                dump(C, "P_o", Pb[:, :, :], [128, 16, 512], BF16)
                raise StopBuild()
            ntt = NT if o == 0 else NOT_
            for tt in range(ntt):
                tb_ = load_tab(TB, I.tabB, tt)
                xm = x1t[tt % 3][:, :]
                S.dma("sp", xm, HY.v(HY.h.ap()[1 + o, tt * 128:(tt + 1) * 128, cs0:cs0 + 512], 2 * (1 + o) + cb, 2 * (1 + o) + cb + 1),
                      "x1t%d" % (tt % 3))
                pi = nextps(C)
                pv = C.ps[pi][:, 0:512]

                def mm(e, pv=pv, tb_=tb_):
                    for ft in range(16):
                        e.matmul(pv.ap, tb_[:, 0, ft, :].ap, Pb[:, ft, :].ap, start=(ft == 0), stop=False)
                    for ft in range(16):
                        ins = e.matmul(pv.ap, tb_[:, 1, ft, :].ap, Qb[:, ft, :].ap, start=False, stop=(ft == 15))
                    return ins
                S.op("pe", mm, R=[tb_[:, :, :, :], Pb[:, :, :], Qb[:, :, :]], W=[pv])
                if o == 0:
                    S.op("dve", lambda e, pv=pv, xm=xm, tt=tt: e.tensor_tensor(vz[:, tt, :].ap, pv.ap, xm.ap, ALU.mult),
                         R=[pv, xm], W=[vz[:, tt, :]])
                else:
                    zv = zz[:, tt, :]
                    S.op("dve", lambda e, pv=pv, xm=xm, zv=zv: e.tensor_tensor(zv.ap, pv.ap, xm.ap, ALU.mult),
                         R=[pv, xm], W=[zv])
                    if "c4a" in C.dbg:
                        continue
                    for g in range(4):
                        S.op("dve", lambda e, g=g, zv=zv, tt=tt: e.bn_stats(gst[:, g, :].ap, zz[:, tt, g * 128:(g + 1) * 128].ap),
                             R=[zv], W=[gst[:, :, :]])
                        S.op("dve", lambda e, g=g: e.bn_aggr(gag[:, g, :].ap, gst[:, g, :].ap), R=[gst[:, :, :]], W=[gag[:, :, :]])
                    S.op("act", lambda e: e.activation(out=grs[:, :].ap, in_=gag[:, :, 1].ap, func=AF.Sqrt, bias=C.eps[:, :].ap),
                         R=[gag[:, :, :], C.eps[:, :]], W=[grs[:, :]])
                    S.op("dve", lambda e: e.reciprocal(out=grs[:, :].ap, in_=grs[:, :].ap), R=[grs[:, :]], W=[grs[:, :]])
                    for g in range(4):
                        S.op("dve", lambda e, g=g, tt=tt, zv=zv: e.tensor_scalar(
                            yn[:, g * 128:(g + 1) * 128].ap, zz[:, tt, g * 128:(g + 1) * 128].ap,
                            gag[:, g, 0:1].ap, grs[:, g:g + 1].ap, ALU.subtract, ALU.mult),
                            R=[zv, gag[:, :, :], grs[:, :]], W=[yn[:, :]])
                    S.op("pool", lambda e, tt=tt, cs0=cs0: e.tensor_tensor(ystage[:, tt, :].ap, yn[:, :].ap, hnw[:, cs0:cs0 + 512].ap, ALU.mult),
                         R=[yn[:, :], hnw[:, :]], W=[ystage[:, tt, :]])
            if "c3" in C.dbg:
                dump(C, "z_o", vz[:, :, :], [128, 16, 512], BF16)
                dump(C, "P_o", Pb[:, :, :], [128, 16, 512], BF16)
                raise StopBuild()
            if o == 1 and "c4a" in C.dbg:
                dump(C, "zz_o", zz[:, :, :], [128, 8, 512])
                raise StopBuild()
            if o == 1:
                S.dma("sp", C.ymix.v(C.ymix.h.ap()[:, cs0:cs0 + 512].rearrange("(tt p) c -> p tt c", p=128), cb, cb + 1),
                      ystage[:, :, :], "ystage")
                if "c4" in C.dbg:
                    dump(C, "zz_o", zz[:, :, :], [128, 8, 512])
                    dump(C, "ys_o", ystage[:, :, :], [128, 8, 512], BF16)
                    raise StopBuild()


def phase_d(C, I, stage):
    nc, S, sb = C.nc, C.S, C.sb
    P0 = 8192
    K1 = 1024
    tri = sb([128, 2, 128], F32, 5120)
    S.dma("sp", tri[:, :, :], DBuf(I.tri, "tri").v(I.tri.ap()), "c12")
    SP = sb([128, 16, 16], F32, P0)
    EB = sb([128, 16, 16], F32, P0 + K1)
    AA = sb([128, 16, 16], F32, P0 + 2 * K1)
    EBL = sb([128, 16, 16], F32, P0 + 3 * K1)
    REB = sb([128, 16, 16], F32, P0 + 5 * K1)
    gtmp = sb([128, 16], F32, P0 + 4 * K1)
    qTh = [sb([128, L], BF16, P0 + (8 + 4 * i) * K1) for i in range(4)]
    kTh = [sb([128, L], BF16, P0 + (24 + 4 * i) * K1) for i in range(4)]
    ktk = [sb([128, 16, 128], BF16, P0 + (40 + 4 * i) * K1) for i in range(4)]
    vau = [sb([128, 16, 129], BF16, P0 + 56 * K1 + 4608 * i) for i in range(4)]
    Cf = [sb([128, 129], F32, P0 + (76 + i) * K1) for i in range(8)]
    CTb = [sb([128, 129], BF16, P0 + 84 * K1 + 512 * i) for i in range(8)]
    stmp = [sb([128, 129], F32, P0 + (88 + i) * K1) for i in range(8)]
    vab = [sb([128, 129], BF16, P0 + 96 * K1 + 512 * i) for i in range(16)]
    stm = [sb([128, 128], BF16, P0 + 104 * K1 + 512 * i) for i in range(16)]
    dsc = [sb([128, 8], F32, P0 + 112 * K1 + 512 * i) for i in range(8)]
    hacc = sb([128, 8, 1024], F32, P0 + 120 * K1)
    osg = [sb([128, 1024], F32, P0 + (152 + 4 * i) * K1) for i in range(2)]
    mnw = sb([128, 1024], F32, P0 + 160 * K1)
    ystg = [sb([128, 1024], BF16, P0 + (164 + 2 * i) * K1) for i in range(2)]
    yn2 = sb([128, 1024], F32, P0 + 168 * K1)
    gst2 = sb([128, 8, 6], F32, P0 + 172 * K1)
    gag2 = sb([128, 8, 2], F32, P0 + 172 * K1 + 512)
    grs2 = sb([128, 8], F32, P0 + 173 * K1)
    S.dma("sp", mnw[:, :], DBuf(I.mnw, "mnw").v(I.mnw.ap()), "c13")
    gates = C.gates
    g5 = gates.h.ap().rearrange("p t (d i h) -> p t d i h", d=2, i=2)
    gall = gates[:, :, :]
    for d in range(2):
        S.op("act", lambda e, d=d: e.activation(out=SP[:, :, d * 8:(d + 1) * 8].ap, in_=g5[:, :, d, 1, :], func=AF.Exp, scale=-1.0),
             R=[gall], W=[SP[:, :, :]])
    S.op("act", lambda e: e.activation(out=SP[:, :, :].ap, in_=SP[:, :, :].ap, func=AF.Ln, bias=1.0), R=[SP[:, :, :]], W=[SP[:, :, :]])
    for c in range(16):
        pi = nextps(C)
        pv = C.ps[pi][:, 0:32]
        S.op("pe", lambda e, pv=pv, c=c: e.matmul(pv.ap[:, 0:8], tri[:, 0, :].ap, SP[:, c, 0:8].ap, start=True, stop=True),
             R=[tri[:, :, :], SP[:, :, :]], W=[pv])
        S.op("pe", lambda e, pv=pv, c=c: e.matmul(pv.ap[:, 8:16], tri[:, 1, :].ap, SP[:, c, 8:16].ap, start=True, stop=True),
             R=[tri[:, :, :], SP[:, :, :]], W=[pv])
        S.op("pe", lambda e, pv=pv, c=c: e.matmul(pv.ap[:, 16:32], C.onesf[:, :].ap, SP[:, c, :].ap, start=True, stop=True),
             R=[C.onesf[:, :], SP[:, :, :]], W=[pv])
        S.op("act", lambda e, pv=pv, c=c: e.activation(out=EB[:, c, :].ap, in_=pv.ap[:, 0:16], func=AF.Exp, scale=-1.0),
             R=[pv], W=[EB[:, c, :]])
        S.op("act", lambda e, pv=pv, c=c: e.activation(out=EBL[:, c, :].ap, in_=pv.ap[:, 16:32], func=AF.Exp, scale=-1.0),
             R=[pv], W=[EBL[:, c, :]])
        S.op("act", lambda e, pv=pv, c=c: e.activation(out=REB[:, c, :].ap, in_=pv.ap[:, 0:16], func=AF.Exp),
             R=[pv], W=[REB[:, c, :]])
        S.op("dve", lambda e, pv=pv, c=c: e.tensor_tensor(
            gtmp[:, :].ap.rearrange("p (d h) -> p d h", d=2), pv.ap[:, 0:16].rearrange("p (d h) -> p d h", d=2),
            g5[:, c, :, 0, :], ALU.add), R=[pv, gall], W=[gtmp[:, :]])
        S.op("act", lambda e, c=c: e.activation(out=AA[:, c, :].ap, in_=gtmp[:, :].ap, func=AF.Exp),
             R=[gtmp[:, :]], W=[AA[:, c, :]])

    QT, KT, KTOK, VTOK, OS = C.qT, C.kT, C.ktok, C.vtok, C.osig
    for i in range(4):
        S.op("pool", lambda e, i=i: e.memset(vau[i][:, :, 128:129].ap, 1.0), W=[vau[i][:, :, :]])
    for grp in range(2):
        for i in range(4):
            hh = grp * 4 + i
            S.dma("sp", qTh[i][:, :], QT.v(QT.h.ap()[hh * 128:(hh + 1) * 128, :], hh, hh + 1), "qTh%d" % i)
            S.dma("sp", kTh[i][:, :], KT.v(KT.h.ap()[hh * 128:(hh + 1) * 128, :], hh, hh + 1), "kTh%d" % i)
            S.dma("sp", ktk[i][:, :, :], KTOK.v(KTOK.h.ap()[:, hh * 128:(hh + 1) * 128].rearrange("(c p) k -> p c k", p=128),
                                                 hh // 4, hh // 4 + 1), "ktk%d" % i)
            S.dma("sp", vau[i][:, :, 0:128], VTOK.v(VTOK.h.ap()[:, hh * 128:(hh + 1) * 128].rearrange("(c p) k -> p c k", p=128),
                                                    hh // 4, hh // 4 + 1), "vau%d" % i)
        for ch in range(8):
            S.op("pool", lambda e, ch=ch: e.memset(Cf[ch][:, :].ap, 0.0), W=[Cf[ch][:, :]])
            S.op("pool", lambda e, ch=ch: e.memset(CTb[ch][:, :].ap, 0.0), W=[CTb[ch][:, :]])
        vai = [0] * 8
        for step in range(16):
            for ch in range(8):
                i, d = ch % 4, ch // 4
                hh = grp * 4 + i
                if d == 0:
                    if step >= 8:
                        continue
                    c, full = step, True
                else:
                    c, full = 15 - step, step >= 8
                gcol = d * 8 + hh
                vslot = ch * 2 + (vai[ch] % 2)
                vai[ch] += 1
                va = vab[vslot][:, :]
                S.op("act", lambda e, va=va, i=i, c=c, gcol=gcol: e.activation(
                    out=va.ap, in_=vau[i][:, c, :].ap, func=AF.Copy, scale=AA[:, c, gcol:gcol + 1].ap),
                    R=[vau[i][:, c, :], AA[:, c, :]], W=[va])
                cs_ = slice(c * 128, (c + 1) * 128)
                if full:
                    stv = C.ps[nextps(C)][:, 0:128]
                    S.op("pe", lambda e, stv=stv, i=i, cs_=cs_: e.matmul(stv.ap, kTh[i][:, cs_].ap, qTh[i][:, cs_].ap, start=True, stop=True),
                         R=[kTh[i][:, cs_], qTh[i][:, cs_]], W=[stv])
                last_upd = not ((d == 0 and c == 7) or (d == 1 and c == 0))
                if last_upd:
                    upv = C.ps[nextps(C)][:, 0:129]
                    S.op("pe", lambda e, upv=upv, i=i, c=c, va=va: e.matmul(upv.ap, ktk[i][:, c, :].ap, va.ap, start=True, stop=True),
                         R=[ktk[i][:, c, :], va], W=[upv])
                if full:
                    sm = stm[vslot][:, :]
                    S.op("dve", lambda e, stv=stv, sm=sm, d=d: e.tensor_tensor(sm.ap, stv.ap, tri[:, d, :].ap, ALU.mult),
                         R=[stv, tri[:, :, :]], W=[sm])
                    nv = C.ps[nextps(C)][:, 0:129]

                    def mmn(e, nv=nv, sm=sm, va=va, i=i, cs_=cs_, ch=ch):
                        e.matmul(nv.ap, sm.ap, va.ap, start=True, stop=False)
                        return e.matmul(nv.ap, qTh[i][:, cs_].ap, CTb[ch][:, :].ap, start=False, stop=True)
                    S.op("pe", mmn, R=[sm, va, qTh[i][:, cs_], CTb[ch][:, :]], W=[nv])
                if last_upd:
                    tv = stmp[ch][:, :]
                    S.op("dve", lambda e, tv=tv, ch=ch, upv=upv: e.tensor_tensor(tv.ap, Cf[ch][:, :].ap, upv.ap, ALU.add),
                         R=[Cf[ch][:, :], upv], W=[tv])
                    S.op("dve", lambda e, tv=tv, ch=ch, c=c, gcol=gcol: e.tensor_scalar(
                        Cf[ch][:, :].ap, tv.ap, EBL[:, c, gcol:gcol + 1].ap, None, ALU.mult),
                        R=[tv, EBL[:, c, :]], W=[Cf[ch][:, :]])
                    S.op("act", lambda e, tv=tv, ch=ch, c=c, gcol=gcol: e.activation(
                        out=CTb[ch][:, :].ap, in_=tv.ap, func=AF.Copy, scale=EBL[:, c, gcol:gcol + 1].ap),
                        R=[tv, EBL[:, c, :]], W=[CTb[ch][:, :]])
                if full:
                    ds = dsc[ch]
                    rebv = REB[:, c, gcol:gcol + 1]
                    S.op("act", lambda e, ds=ds, nv=nv: e.activation(out=ds[:, 0:1].ap, in_=nv.ap[:, 128:129], func=AF.Abs),
                         R=[nv], W=[ds[:, :]])
                    S.op("dve", lambda e, ds=ds, rebv=rebv: e.tensor_tensor(ds[:, 4:5].ap, ds[:, 0:1].ap, rebv.ap, ALU.max),
                         R=[ds[:, :], rebv], W=[ds[:, :]])
                    S.op("dve", lambda e, ds=ds: e.reciprocal(out=ds[:, 5:6].ap, in_=ds[:, 4:5].ap), R=[ds[:, :]], W=[ds[:, :]])
                    hv = hacc[:, c, hh * 128:(hh + 1) * 128]
                    if d == 0:
                        S.op("dve", lambda e, hv=hv, nv=nv, ds=ds: e.tensor_scalar(hv.ap, nv.ap[:, 0:128], ds[:, 5:6].ap, None, ALU.mult),
                             R=[nv, ds[:, :]], W=[hv])
                    else:
                        S.op("dve", lambda e, hv=hv, nv=nv, ds=ds: e.scalar_tensor_tensor(
                            hv.ap, nv.ap[:, 0:128], ds[:, 5:6].ap, hv.ap, ALU.mult, ALU.add),
                            R=[nv, ds[:, :], hv], W=[hv])
    for tt in range(8):
        ov = osg[tt % 2][:, :]
        S.dma("sp", ov, OS.v(OS.h.ap()[tt * 128:(tt + 1) * 128, :]), "osg%d" % (tt % 2))
        for hh in range(8):
            hv = hacc[:, tt, hh * 128:(hh + 1) * 128]
            S.op("dve", lambda e, hv=hv, hh=hh: e.bn_stats(gst2[:, hh, :].ap, hv.ap), R=[hv], W=[gst2[:, :, :]])
            S.op("dve", lambda e, hh=hh: e.bn_aggr(gag2[:, hh, :].ap, gst2[:, hh, :].ap), R=[gst2[:, :, :]], W=[gag2[:, :, :]])
        S.op("act", lambda e: e.activation(out=grs2[:, :].ap, in_=gag2[:, :, 1].ap, func=AF.Sqrt, bias=C.eps[:, :].ap),
             R=[gag2[:, :, :], C.eps[:, :]], W=[grs2[:, :]])
        S.op("dve", lambda e: e.reciprocal(out=grs2[:, :].ap, in_=grs2[:, :].ap), R=[grs2[:, :]], W=[grs2[:, :]])
        for hh in range(8):
            hv = hacc[:, tt, hh * 128:(hh + 1) * 128]
            S.op("dve", lambda e, hv=hv, hh=hh: e.tensor_scalar(
                yn2[:, hh * 128:(hh + 1) * 128].ap, hv.ap, gag2[:, hh, 0:1].ap, grs2[:, hh:hh + 1].ap, ALU.subtract, ALU.mult),
                R=[hv, gag2[:, :, :], grs2[:, :]], W=[yn2[:, :]])
        S.op("pool", lambda e: e.tensor_tensor(yn2[:, :].ap, yn2[:, :].ap, mnw[:, :].ap, ALU.mult), R=[yn2[:, :], mnw[:, :]], W=[yn2[:, :]])
        yv = ystg[tt % 2][:, :]
        S.op("dve", lambda e, yv=yv, ov=ov: e.tensor_tensor(yv.ap, yn2[:, :].ap, ov.ap, ALU.mult), R=[yn2[:, :], ov], W=[yv])
        S.dma("sp", C.ymix.v(C.ymix.h.ap()[tt * 128:(tt + 1) * 128, 1024:2048], 2, 4), yv, "ystg%d" % (tt % 2))


def phase_e(C, I, stage):
    nc, S, sb = C.nc, C.S, C.sb
    P0 = 8192
    K1 = 1024
    yacc = sb([128, 8, D], F32, P0)
    C.yacc = yacc
    Wout = sb([128, 16, D], BF16, P0 + 64 * K1)
    ytile = [sb([128, D], BF16, P0 + (128 + 4 * i) * K1) for i in range(2)]
    yT = [sb([128, 16, 128], BF16, P0 + (136 + 4 * i) * K1) for i in range(2)]
    r = sb([128, D], F32, P0 + 144 * K1)
    bout = sb([128, D], F32, P0 + 152 * K1)
    g1 = sb([128, D], F32, P0 + 160 * K1)
    b1 = sb([128, D], F32, P0 + 168 * K1)
    x1T = sb([128, 16, 128], F32, P0 + 176 * K1)
    x1st = sb([128, D], BF16, P0 + 184 * K1)
    Wr = sb([128, 16, NE], F32, P0 + 188 * K1)
    rb = sb([128, NE], F32, P0 + 190 * K1)
    lg = sb([128, NE], F32, P0 + 190 * K1 + 512)
    m8 = sb([128, 8], F32, P0 + 191 * K1)
    sm = sb([128, 8], F32, P0 + 191 * K1 + 512)
    ex = sb([128, NE], F32, P0 + 192 * K1)
    mk = sb([128, NE], F32, P0 + 192 * K1 + 512)
    lst = sb([128, 4, 6], F32, P0 + 193 * K1)
    lag_ = sb([128, 2], F32, P0 + 193 * K1 + 128)
    lrs = sb([128, 1], F32, P0 + 193 * K1 + 192)
    C.G = sb([128, 8, NE], F32, 6144)
    C.POS = sb([128, 8, NE], F32, 7168)
    C.x1s = C.dscr("x1s", [OWN, D], BF16, 8)
    WO = DBuf(I.w_out, "w_out")
    for q in range(4):
        S.dma("pool", Wout[:, q * 4:(q + 1) * 4, :],
              WO.v(I.w_out.ap()[q * 512:(q + 1) * 512, :].rearrange("(ct p) d -> p ct d", p=128)), "wout%d" % q)
    for nm, dst, h in (("boutb", bout, I.boutb), ("ln1g", g1, I.ln1g), ("ln1b", b1, I.ln1b), ("rbb", rb, I.rbb)):
        S.dma("sp", dst[:, :], DBuf(h, nm).v(h.ap()), "e_" + nm)
    S.dma("sp", Wr[:, :, :], DBuf(I.router_w, "router_w").v(I.router_w.ap().rearrange("(kt p) e -> p kt e", p=128)), "e_wr")
    X = DBuf(I.x, "x")
    YM = C.ymix
    for tt in range(8):
        yt = ytile[tt % 2]
        yTt = yT[tt % 2]
        S.dma("sp", yt[:, :], YM.v(YM.h.ap()[tt * 128:(tt + 1) * 128, :]), "ytile%d" % (tt % 2))
        ya = yacc[:, tt, :]
        S.dma("sp", ya, X.v(I.x.ap()[tt * 128:(tt + 1) * 128, :]), "xres")
        for hb in range(2):
            pi = nextps(C)
            pb = C.psb[pi]
            for j in range(8):
                ct = hb * 8 + j
                S.op("pe", lambda e, pb=pb, yt=yt, j=j, ct=ct: e.transpose(
                    pb[:, j * 128:(j + 1) * 128].ap, yt[:, ct * 128:(ct + 1) * 128].ap, C.idb[:, :].ap),
                    R=[yt[:, ct * 128:(ct + 1) * 128], C.idb[:, :]], W=[pb[:, j * 128:(j + 1) * 128]])
            dst = yTt[:, hb * 8:(hb + 1) * 8, :]
            S.op("act", lambda e, pb=pb, dst=dst: e.copy(out=dst.ap, in_=pb[:, :].ap.rearrange("p (a b) -> p a b", a=8)),
                 R=[pb[:, :]], W=[dst])
        for db in range(4):
            pi = nextps(C)
            pv = C.ps[pi][:, 0:512]

            def mm(e, pv=pv, yTt=yTt, db=db):
                for ct in range(16):
                    ins = e.matmul(pv.ap, yTt[:, ct, :].ap, Wout[:, ct, db * 512:(db + 1) * 512].ap, start=(ct == 0), stop=(ct == 15))
                return ins
            S.op("pe", mm, R=[yTt[:, :, :], Wout[:, :, db * 512:(db + 1) * 512]], W=[pv])
            rv = r[:, db * 512:(db + 1) * 512]
            S.op("dve", lambda e, pv=pv, rv=rv, db=db: e.tensor_tensor(rv.ap, pv.ap, bout[:, db * 512:(db + 1) * 512].ap, ALU.add),
                 R=[pv, bout[:, :]], W=[rv])
        S.op("dve", lambda e, ya=ya: e.scalar_tensor_tensor(r[:, :].ap, ya.ap, DN_ALPHA, r[:, :].ap, ALU.mult, ALU.add),
             R=[ya, r[:, :]], W=[r[:, :]])
        layer_norm_tile(C, r, lst, lag_, lrs, g1, b1)
        S.op("act", lambda e, ya=ya: e.mul(ya.ap, r[:, :].ap, DN_ALPHA), R=[r[:, :]], W=[ya])
        S.op("act", lambda e: e.copy(out=x1st[:, :].ap, in_=r[:, :].ap), R=[r[:, :]], W=[x1st[:, :]])
        S.dma("sp", C.x1s.v(C.x1s.h.ap()[tt * 128:(tt + 1) * 128, :], tt, tt + 1), x1st[:, :], "x1st")
        for qd in range(4):
            pi = nextps(C)
            pf = C.ps[pi]
            for j in range(4):
                kt = qd * 4 + j
                S.op("pe", lambda e, pf=pf, j=j, kt=kt: e.transpose(
                    pf[:, j * 128:(j + 1) * 128].ap, r[:, kt * 128:(kt + 1) * 128].ap, C.idf[:, :].ap),
                    R=[r[:, kt * 128:(kt + 1) * 128], C.idf[:, :]], W=[pf[:, j * 128:(j + 1) * 128]])
            dst = x1T[:, qd * 4:(qd + 1) * 4, :]
            S.op("dve", lambda e, pf=pf, dst=dst: e.tensor_copy(out=dst.ap, in_=pf[:, :].ap.rearrange("p (a b) -> p a b", a=4)),
                 R=[pf[:, :]], W=[dst])
        pi = nextps(C)
        pl = C.ps[pi][:, 0:NE]

        def mmr(e, pl=pl):
            for kt in range(16):
                ins = e.matmul(pl.ap, x1T[:, kt, :].ap, Wr[:, kt, :].ap, start=(kt == 0), stop=(kt == 15))
            return ins
        S.op("pe", mmr, R=[x1T[:, :, :], Wr[:, :, :]], W=[pl])
        S.op("dve", lambda e, pl=pl: e.tensor_tensor(lg[:, :].ap, pl.ap, rb[:, :].ap, ALU.add), R=[pl, rb[:, :]], W=[lg[:, :]])
        S.op("dve", lambda e: e.max(out=m8[:, :].ap, in_=lg[:, :].ap), R=[lg[:, :]], W=[m8[:, :]])
        S.op("dve", lambda e: e.tensor_scalar(mk[:, :].ap, lg[:, :].ap, m8[:, 3:4].ap, None, ALU.is_ge), R=[lg[:, :], m8[:, :]], W=[mk[:, :]])
        S.op("dve", lambda e: e.tensor_scalar(sm[:, 0:1].ap, m8[:, 0:1].ap, -1.0, None, ALU.mult), R=[m8[:, :]], W=[sm[:, :]])
        S.op("act", lambda e: e.activation(out=ex[:, :].ap, in_=lg[:, :].ap, func=AF.Exp, bias=sm[:, 0:1].ap), R=[lg[:, :], sm[:, :]], W=[ex[:, :]])
        S.op("dve", lambda e: e.tensor_tensor(ex[:, :].ap, ex[:, :].ap, mk[:, :].ap, ALU.mult), R=[ex[:, :], mk[:, :]], W=[ex[:, :]])
        S.op("dve", lambda e: e.reduce_sum(out=sm[:, 1:2].ap, in_=ex[:, :].ap, axis=AX.X), R=[ex[:, :]], W=[sm[:, :]])
        S.op("dve", lambda e: e.reciprocal(out=sm[:, 2:3].ap, in_=sm[:, 1:2].ap), R=[sm[:, :]], W=[sm[:, :]])
        gv = C.G[:, tt, :]
        S.op("dve", lambda e, gv=gv: e.tensor_scalar(gv.ap, ex[:, :].ap, sm[:, 2:3].ap, None, ALU.mult), R=[ex[:, :], sm[:, :]], W=[gv])


def layer_norm_tile(C, r, lst, lag_, lrs, g, b):
    S = C.S
    for j in range(4):
        S.op("dve", lambda e, j=j: e.bn_stats(lst[:, j, :].ap, r[:, j * 512:(j + 1) * 512].ap), R=[r[:, :]], W=[lst[:, :, :]])
    S.op("dve", lambda e: e.bn_aggr(lag_[:, :].ap, lst[:, :, :].ap.rearrange("p a b -> p (a b)")), R=[lst[:, :, :]], W=[lag_[:, :]])
    S.op("act", lambda e: e.activation(out=lrs[:, :].ap, in_=lag_[:, 1:2].ap, func=AF.Sqrt, bias=C.eps[:, :].ap),
         R=[lag_[:, :], C.eps[:, :]], W=[lrs[:, :]])
    S.op("dve", lambda e: e.reciprocal(out=lrs[:, :].ap, in_=lrs[:, :].ap), R=[lrs[:, :]], W=[lrs[:, :]])
    S.op("dve", lambda e: e.tensor_scalar(r[:, :].ap, r[:, :].ap, lag_[:, 0:1].ap, lrs[:, :].ap, ALU.subtract, ALU.mult),
         R=[r[:, :], lag_[:, :], lrs[:, :]], W=[r[:, :]])
    S.op("pool", lambda e: e.tensor_tensor(r[:, :].ap, r[:, :].ap, g[:, :].ap, ALU.mult), R=[r[:, :], g[:, :]], W=[r[:, :]])
    S.op("pool", lambda e: e.tensor_tensor(r[:, :].ap, r[:, :].ap, b[:, :].ap, ALU.add), R=[r[:, :], b[:, :]], W=[r[:, :]])


def phase_f(C, I, stage, ne):
    nc, S, sb = C.nc, C.S, C.sb
    P0 = 8192
    K1 = 1024
    yacc = C.yacc
    G, POS = C.G, C.POS
    x1bf = sb([128, 8, D], BF16, P0 + 64 * K1)
    NRING = 4
    Wring = [sb([128, 16, 512], BF16, P0 + (96 + 16 * i) * K1) for i in range(NRING)]
    Wring4 = [sb([128, 2, 16, 256], BF16, P0 + (96 + 16 * i) * K1) for i in range(NRING)]
    xgT = sb([128, 16, CAP], BF16, P0 + 160 * K1)
    actT = sb([128, 16, CAP], BF16, P0 + 168 * K1)
    sel = sb([128, 8, CAP], BF16, P0 + 176 * K1)
    selT = sb([128, 2, OWN], BF16, P0 + 180 * K1)
    oe = [sb([128, 2, 512], BF16, P0 + (184 + 2 * i) * K1) for i in range(2)]
    tg = [sb([128, CAP], F32, P0 + (188 + i) * K1) for i in range(2)]
    ts_ = [sb([128, CAP], F32, P0 + (190 + i) * K1) for i in range(2)]
    tu = [sb([128, CAP], F32, P0 + (192 + i) * K1) for i in range(2)]
    bgu = [sb([128, 32], F32, P0 + 194 * K1 + 128 * i) for i in range(2)]
    iota = sb([128, CAP], F32, P0 + 195 * K1)
    Mall = sb([128, 8, NE], F32, P0 + 196 * K1)
    cnt = sb([128, 8, NE], F32, P0 + 197 * K1)
    sut = sb([128, 128], F32, P0 + 198 * K1)
    GT = sb([32, OWN], F32, P0 + 96 * K1)
    bd = sb([32, D], F32, P0 + 100 * K1)
    S.dma("sp", iota[:, :], DBuf(I.iota, "iota").v(I.iota.ap()), "f_iota")
    S.dma("sp", sut[:, :], DBuf(I.sutri, "sutri").v(I.sutri.ap()), "f_sut")
    S.dma("sp", bd[:, :], DBuf(I.bdown, "bdown").v(I.bdown.ap()), "f_bd")
    S.dma("sp", x1bf[:, :, :], C.x1s.v(C.x1s.h.ap().rearrange("(tt p) d -> p tt d", p=128)), "f_x1bf")
    S.op("dve", lambda e: e.tensor_scalar(Mall[:, :, :].ap, G[:, :, :].ap, 0.0, None, ALU.is_gt), R=[G[:, :, :]], W=[Mall[:, :, :]])
    pi = nextps(C)
    pw = C.ps[pi][:, 0:256]
    S.op("pe", lambda e, pw=pw: e.matmul(pw.ap, sut[:, :].ap, Mall[:, :, :].ap.rearrange("p a b -> p (a b)"), start=True, stop=True),
         R=[sut[:, :], Mall[:, :, :]], W=[pw])
    pi = nextps(C)
    pc = C.ps[pi][:, 0:256]
    S.op("pe", lambda e, pc=pc: e.matmul(pc.ap, C.onesf[:, :].ap, Mall[:, :, :].ap.rearrange("p a b -> p (a b)"), start=True, stop=True),
         R=[C.onesf[:, :], Mall[:, :, :]], W=[pc])
    S.op("dve", lambda e, pc=pc: e.tensor_copy(out=cnt[:, :, :].ap.rearrange("p a b -> p (a b)"), in_=pc.ap), R=[pc], W=[cnt[:, :, :]])
    S.op("dve", lambda e, pw=pw: e.tensor_copy(out=POS[:, :, :].ap.rearrange("p a b -> p (a b)"), in_=pw.ap), R=[pw], W=[POS[:, :, :]])
    for tt in range(1, 8):
        pass
    S.op("dve", lambda e: e.tensor_copy(out=Mall[:, 0, :].ap, in_=cnt[:, 0, :].ap), R=[cnt[:, :, :]], W=[Mall[:, :, :]])
    for tt in range(1, 8):
        S.op("dve", lambda e, tt=tt: e.tensor_tensor(POS[:, tt, :].ap, POS[:, tt, :].ap, Mall[:, 0, :].ap, ALU.add),
             R=[POS[:, :, :], Mall[:, :, :]], W=[POS[:, :, :]])
        if tt < 7:
            S.op("dve", lambda e, tt=tt: e.tensor_tensor(Mall[:, 0, :].ap, Mall[:, 0, :].ap, cnt[:, tt, :].ap, ALU.add),
                 R=[cnt[:, :, :], Mall[:, :, :]], W=[Mall[:, :, :]])
    S.op("dve", lambda e: e.tensor_scalar(Mall[:, :, :].ap, G[:, :, :].ap, 0.0, None, ALU.is_gt), R=[G[:, :, :]], W=[Mall[:, :, :]])
    for tt in range(8):
        pi = nextps(C)
        pg = C.ps[pi][0:32, 0:128]
        S.op("pe", lambda e, pg=pg, tt=tt: e.transpose(pg.ap, G[:, tt, :].ap, C.idf[:, :].ap), R=[G[:, tt, :], C.idf[:, :]], W=[pg])
        S.op("act", lambda e, pg=pg, tt=tt: e.copy(out=GT[:, tt * 128:(tt + 1) * 128].ap, in_=pg.ap), R=[pg], W=[GT[:, tt * 128:(tt + 1) * 128]])
    for tt in range(8):
        for db in range(4):
            pi = nextps(C)
            pv = C.ps[pi][:, 0:512]
            S.op("pe", lambda e, pv=pv, tt=tt, db=db: e.matmul(pv.ap, GT[:, tt * 128:(tt + 1) * 128].ap, bd[:, db * 512:(db + 1) * 512].ap, start=True, stop=True),
                 R=[GT[:, :], bd[:, :]], W=[pv])
            yv = yacc[:, tt, db * 512:(db + 1) * 512]
            S.op("dve", lambda e, pv=pv, yv=yv: e.tensor_tensor(yv.ap, yv.ap, pv.ap, ALU.add), R=[pv, yv], W=[yv])
    WG = DBuf(I.w_gu, "w_gu")
    WDn = DBuf(I.w_down, "w_down")
    BG = DBuf(I.bgu, "bgu")
    gi = [0]
    di = [0]
    for ex_ in range(ne):
        bg = bgu[ex_ % 2]
        S.dma("sp", bg[:, :], BG.v(I.bgu.ap()[ex_]), "f_bgu%d" % (ex_ % 2))
        for tt in range(8):
            S.op("dve", lambda e, tt=tt, ex_=ex_: e.tensor_scalar(
                sel[:, tt, :].ap, iota[:, :].ap, POS[:, tt, ex_:ex_ + 1].ap, Mall[:, tt, ex_:ex_ + 1].ap, ALU.is_equal, ALU.mult),
                R=[iota[:, :], POS[:, tt, :], Mall[:, tt, :]], W=[sel[:, tt, :]])
        for kt in range(16):
            pi = nextps(C)
            pv = C.ps[pi][:, 0:CAP]

            def mmg(e, pv=pv, kt=kt):
                for tt in range(8):
                    ins = e.matmul(pv.ap, x1bf[:, tt, kt * 128:(kt + 1) * 128].ap, sel[:, tt, :].ap, start=(tt == 0), stop=(tt == 7))
                return ins
            S.op("pe", mmg, R=[x1bf[:, :, kt * 128:(kt + 1) * 128], sel[:, :, :]], W=[pv])
            if kt % 2 == 0:
                S.op("act", lambda e, pv=pv, kt=kt: e.copy(out=xgT[:, kt, :].ap, in_=pv.ap), R=[pv], W=[xgT[:, kt, :]])
            else:
                S.op("dve", lambda e, pv=pv, kt=kt: e.tensor_copy(out=xgT[:, kt, :].ap, in_=pv.ap), R=[pv], W=[xgT[:, kt, :]])
        for jt in range(2):
            pi = nextps(C)
            pb = C.psb[pi]
            for tt in range(8):
                S.op("pe", lambda e, pb=pb, tt=tt, jt=jt: e.transpose(
                    pb[:, tt * 128:(tt + 1) * 128].ap, sel[:, tt, jt * 128:(jt + 1) * 128].ap, C.idb[:, :].ap),
                    R=[sel[:, tt, :], C.idb[:, :]], W=[pb[:, tt * 128:(tt + 1) * 128]])
            S.op("act", lambda e, pb=pb, jt=jt: e.copy(out=selT[:, jt, :].ap, in_=pb[:, :].ap), R=[pb[:, :]], W=[selT[:, jt, :]])
        for fb in range(8):
            slot = gi[0] % NRING
            gi[0] += 1
            wg = Wring[slot]
            wg4 = Wring4[slot]
            S.dma("pool", wg4[:, 0, :, :], WG.v(I.w_gu.ap()[ex_, :, fb * 256:(fb + 1) * 256].rearrange("(kt p) f -> p kt f", p=128)),
                  "wgu%da" % slot)
            S.dma("pool", wg4[:, 1, :, :], WG.v(I.w_gu.ap()[ex_, :, 2048 + fb * 256:2048 + (fb + 1) * 256].rearrange("(kt p) f -> p kt f", p=128)),
                  "wgu%db" % slot)
            for fl in range(2):
                ft = fb * 2 + fl
                pig = nextps(C)
                pgv = C.ps[pig][:, 0:CAP]
                piu = nextps(C)
                puv = C.ps[piu][:, 0:CAP]
                for (pvv, half) in ((pgv, 0), (puv, 1)):
                    def mmu(e, pvv=pvv, half=half, wg4=wg4, fl=fl):
                        for kt in range(16):
                            ins = e.matmul(pvv.ap, wg4[:, half, kt, fl * 128:(fl + 1) * 128].ap, xgT[:, kt, :].ap,
                                           start=(kt == 0), stop=(kt == 15))
                        return ins
                    S.op("pe", mmu, R=[wg4[:, half, :, :], xgT[:, :, :]], W=[pvv])
                a = ft % 2
                S.op("dve", lambda e, pgv=pgv, a=a, bg=bg, ft=ft: e.tensor_scalar(
                    tg[a][:, :].ap, pgv.ap, bg[:, ft:ft + 1].ap, 7.0, ALU.add, ALU.min), R=[pgv, bg[:, :]], W=[tg[a][:, :]])
                S.op("act", lambda e, a=a: e.activation(out=ts_[a][:, :].ap, in_=tg[a][:, :].ap, func=AF.Sigmoid, scale=1.702),
                     R=[tg[a][:, :]], W=[ts_[a][:, :]])
                S.op("dve", lambda e, puv=puv, a=a, bg=bg, ft=ft: e.tensor_scalar(
                    tu[a][:, :].ap, puv.ap, bg[:, 16 + ft:17 + ft].ap, 7.0, ALU.add, ALU.min), R=[puv, bg[:, :]], W=[tu[a][:, :]])
                S.op("dve", lambda e, a=a: e.tensor_scalar(tu[a][:, :].ap, tu[a][:, :].ap, -7.0, 1.0, ALU.max, ALU.add),
                     R=[tu[a][:, :]], W=[tu[a][:, :]])
                S.op("dve", lambda e, a=a: e.tensor_tensor(tg[a][:, :].ap, tg[a][:, :].ap, ts_[a][:, :].ap, ALU.mult),
                     R=[tg[a][:, :], ts_[a][:, :]], W=[tg[a][:, :]])
                S.op("dve", lambda e, a=a, ft=ft: e.tensor_tensor(actT[:, ft, :].ap, tg[a][:, :].ap, tu[a][:, :].ap, ALU.mult),
                     R=[tg[a][:, :], tu[a][:, :]], W=[actT[:, ft, :]])
        def down_block(db):
            slot = gi[0] % NRING
            gi[0] += 1
            wd = Wring[slot]
            S.dma("pool", wd[:, :, :], WDn.v(I.w_down.ap()[ex_, :, db * 512:(db + 1) * 512].rearrange("(ft p) d -> p ft d", p=128)),
                  "wgu%da" % slot)
            oev = oe[db % 2]
            for jt in range(2):
                pi = nextps(C)
                pv = C.ps[pi][:, 0:512]

                def mmd(e, pv=pv, wd=wd, jt=jt):
                    for ft in range(16):
                        ins = e.matmul(pv.ap, actT[:, ft, jt * 128:(jt + 1) * 128].ap, wd[:, ft, :].ap, start=(ft == 0), stop=(ft == 15))
                    return ins
                S.op("pe", mmd, R=[actT[:, :, :], wd[:, :, :]], W=[pv])
                S.op("act", lambda e, pv=pv, oev=oev, jt=jt: e.copy(out=oev[:, jt, :].ap, in_=pv.ap), R=[pv], W=[oev[:, jt, :]])

        def scatter_block(db, ex_=ex_):
            oev = oe[db % 2]
            for tt in range(8):
                pi = nextps(C)
                pv = C.ps[pi][:, 0:512]

                def mms(e, pv=pv, oev=oev, tt=tt):
                    e.matmul(pv.ap, selT[:, 0, tt * 128:(tt + 1) * 128].ap, oev[:, 0, :].ap, start=True, stop=False)
                    return e.matmul(pv.ap, selT[:, 1, tt * 128:(tt + 1) * 128].ap, oev[:, 1, :].ap, start=False, stop=True)
                S.op("pe", mms, R=[selT[:, :, tt * 128:(tt + 1) * 128], oev[:, :, :]], W=[pv])
                yv = yacc[:, tt, db * 512:(db + 1) * 512]
                S.op("dve", lambda e, pv=pv, yv=yv, tt=tt, ex_=ex_: e.scalar_tensor_tensor(
                    yv.ap, pv.ap, G[:, tt, ex_:ex_ + 1].ap, yv.ap, ALU.mult, ALU.add), R=[pv, yv, G[:, tt, :]], W=[yv])

        down_block(0)
        for db in range(1, 4):
            down_block(db)
            scatter_block(db - 1)
        scatter_block(3)


def phase_g(C, I, stage):
    nc, S, sb = C.nc, C.S, C.sb
    P0 = 8192
    K1 = 1024
    yacc = C.yacc
    g2 = sb([128, D], F32, P0 + 64 * K1)
    b2 = sb([128, D], F32, P0 + 72 * K1)
    r = [sb([128, D], F32, P0 + (80 + 8 * i) * K1) for i in range(2)]
    lst = sb([128, 4, 6], F32, P0 + 96 * K1)
    lag_ = sb([128, 2], F32, P0 + 96 * K1 + 128)
    lrs = sb([128, 1], F32, P0 + 96 * K1 + 192)
    S.dma("sp", g2[:, :], DBuf(I.ln2g, "ln2g").v(I.ln2g.ap()), "g_g2")
    S.dma("sp", b2[:, :], DBuf(I.ln2b, "ln2b").v(I.ln2b.ap()), "g_b2")
    O = DBuf(I.out, "out", 8)
    for tt in range(8):
        rr = r[tt % 2]
        S.op("act", lambda e, rr=rr, tt=tt: e.copy(out=rr[:, :].ap, in_=yacc[:, tt, :].ap), R=[yacc[:, tt, :]], W=[rr[:, :]])
        layer_norm_tile(C, rr, lst, lag_, lrs, g2, b2)
        S.dma("sp", O.v(I.out.ap()[tt * 128:(tt + 1) * 128, :], tt, tt + 1), rr[:, :], "out%d" % (tt % 2))

def from_phases(C, stage, ne, dbg):
    I = ext_inputs(C, ne, stage)
    C.I = I
    phase_ab(C, I, stage)
    if stage >= 2 and "skipc" not in dbg:
        phase_c(C, I, stage)
    if stage >= 3:
        if "skipc" in dbg:
            C.ymix = C.dscr("ymix", [OWN, 2048], BF16, 4)
            C.eps = C.sb([128, 1], F32, 4128)
            C.onesf = C.sb([128, 128], F32, 4608)
            C.S.op("dve", lambda e: e.memset(C.eps[:, :].ap, LN_EPS), W=[C.eps[:, :]])
            C.S.op("dve", lambda e: e.memset(C.onesf[:, :].ap, 1.0), W=[C.onesf[:, :]])
        phase_d(C, I, stage)
    if stage >= 4:
        if "skipd" in dbg:
            C.ymix = getattr(C, "ymix", None) or C.dscr("ymix", [OWN, 2048], BF16, 4)
        phase_e(C, I, stage)
    if "G_o" in dbg:
        dump(C, "G_o", C.G[:, :, :], [128, 8, NE])
    if stage >= 5:
        phase_f(C, I, stage, ne)
        phase_g(C, I, stage)
    if "gates" in dbg:
        g = C.nc.dram_tensor("gates_o", [128, 16, 32], F32, kind="ExternalOutput")
        C.S.dma("sp", DBuf(g, "gates_o").v(g.ap()), C.gates[:, :, :], "dbg_g")


def prep_core_ab(inp, b, hf):
    rev = hf == 1
    x = inp["x"][b]
    if rev:
        x = x[::-1]
    w_in = inp["w_in"][0]
    b_in = inp["b_in"][0]
    if rev:
        gperm = np.concatenate([np.arange(7168), np.arange(7184, 7200), np.arange(7168, 7184)])
        w_in = w_in[:, gperm]
        b_in = b_in[gperm]
    hcw = inp["hy_conv_w"][0]
    mcw = inp["ml_conv_w"][0]
    if rev:
        hcw = hcw[::-1]
        mcw = mcw[::-1]
    cw = np.concatenate([hcw, mcw], axis=1)
    cb = np.concatenate([inp["hy_conv_b"][0], inp["ml_conv_b"][0]])
    convw = np.stack([cw[0], cw[1], cw[2], cb, b_in[:5120]], axis=-1)
    convw = convw.reshape(40, 128, 5).transpose(1, 0, 2)
    m = {
        "x": np.ascontiguousarray(x, dtype=np.float32),
        "w_in": np.ascontiguousarray(w_in, dtype=np.float32),
        "convw": np.ascontiguousarray(convw, dtype=np.float32),
        "bias_tok": np.ascontiguousarray(np.broadcast_to(b_in[5120:7200], (128, 2080)), dtype=np.float32),
        "idb": np.eye(128, dtype=np.float32).astype(ml_dtypes.bfloat16),
        "idf": np.eye(128, dtype=np.float32),
    }
    return m


_CONST_CACHE = {}


def hyena_consts():
    if "hy" in _CONST_CACHE:
        return _CONST_CACHE["hy"]
    t = np.linspace(0.0, 1.0, L, dtype=np.float64)
    bands = 16
    fb = np.linspace(1e-4, bands - 1, bands, dtype=np.float64)[None]
    w = 2.0 * np.pi * np.arange(L, dtype=np.float64)[:, None] / L
    z = np.concatenate([t[:, None], np.cos(fb * w), -np.sin(fb * w)], -1)
    zT = np.ascontiguousarray(z.T).astype(np.float32)
    deltas = np.abs(np.linspace(np.log(1e-2) / 1.5, np.log(1e-2) / 0.3, DH, dtype=np.float64))
    decay = np.exp(-t[:, None] * deltas[None]).astype(np.float32)
    n = np.arange(2048, dtype=np.float64)
    ang = 2.0 * np.pi * np.outer(n, n + 0.5) / 4096.0
    G = np.stack([np.cos(ang), np.sin(ang)], 0)
    tabA = G.reshape(2, 16, 128, 16, 128).transpose(3, 2, 0, 1, 4)
    tabB = G.reshape(2, 16, 128, 16, 128).transpose(1, 4, 0, 3, 2)
    tabA = np.ascontiguousarray(tabA).astype(np.float32).astype(ml_dtypes.bfloat16)
    tabB = np.ascontiguousarray(tabB).astype(np.float32).astype(ml_dtypes.bfloat16)
    _CONST_CACHE["hy"] = dict(zT=zT, decay=decay, tabA=tabA, tabB=tabB)
    return _CONST_CACHE["hy"]


def prep_core_c(inp, b, hf):
    rev = hf == 1
    m = dict(hyena_consts())
    w3 = inp["hy_filt_w3"][0]
    if rev:
        w3 = w3.reshape(64, 2, 2, DH)[:, :, ::-1, :].reshape(64, 4096)
    m["fw1"] = np.ascontiguousarray(inp["hy_filt_w1"][0], dtype=np.float32)
    m["fw2"] = np.ascontiguousarray(inp["hy_filt_w2"][0], dtype=np.float32)
    m["fw3"] = np.ascontiguousarray(w3, dtype=np.float32)
    fr = inp["hy_filt_freq"][0]
    m["fsm"] = np.ascontiguousarray(np.stack([inp["hy_filt_b1"][0], inp["hy_filt_b2"][0], fr[0], fr[1]], -1), dtype=np.float32)
    m["skipb"] = np.ascontiguousarray(np.broadcast_to(inp["hy_skip"][0][None], (128, 2, DH)), dtype=np.float32)
    m["hnw"] = np.ascontiguousarray(np.broadcast_to(inp["hy_norm_w"][0][None], (128, DH)), dtype=np.float32)
    lm = np.ones((128, 2), np.float32)
    lm[0, 0] = 0.0 if rev else 1.0
    lm[0, 1] = 1.0 if rev else 0.0
    m["lagmask"] = lm
    return m


def prep_core_d(inp, b, hf):
    p = np.arange(128)
    U = (p[:, None] <= p[None, :]).astype(np.float32)
    Lm = (p[:, None] >= p[None, :]).astype(np.float32)
    return {
        "tri": np.ascontiguousarray(np.stack([U, Lm], 1)),
        "mnw": np.ascontiguousarray(np.broadcast_to(inp["ml_norm_w"][0][None], (128, 1024)), dtype=np.float32),
    }


def prep_core_efg(inp, b, hf, ne=NE):
    bc = lambda a, n: np.ascontiguousarray(np.broadcast_to(np.asarray(a, np.float32)[None], (128, n)), dtype=np.float32)
    p = np.arange(128)
    m = {
        "w_out": np.ascontiguousarray(inp["w_out"][0], dtype=np.float32),
        "boutb": bc(inp["b_out"][0], D),
        "ln1g": bc(inp["ln1_g"][0], D),
        "ln1b": bc(inp["ln1_b"][0], D),
        "ln2g": bc(inp["ln2_g"][0], D),
        "ln2b": bc(inp["ln2_b"][0], D),
        "router_w": np.ascontiguousarray(inp["router_w"][0], dtype=np.float32),
        "rbb": bc(inp["router_b"][0], NE),
        "sutri": (p[:, None] < p[None, :]).astype(np.float32),
        "iota": np.ascontiguousarray(np.broadcast_to(np.arange(CAP, dtype=np.float32)[None], (128, CAP))),
        "bdown": np.ascontiguousarray(inp["b_down"][0], dtype=np.float32),
    }
    if "w_gu" in inp:
        m["w_gu"] = inp["w_gu"][0][:ne]
        m["w_down"] = inp["w_down"][0][:ne]
        m["bgu"] = np.ascontiguousarray(inp["b_gu"][0][:ne].reshape(ne, 32, 128).transpose(0, 2, 1), dtype=np.float32)
    return m


def prep_core(inp, b, hf, ne=NE):
    m = prep_core_ab(inp, b, hf)
    m.update(prep_core_c(inp, b, hf))
    m.update(prep_core_d(inp, b, hf))
    m.update(prep_core_efg(inp, b, hf, ne))
    return m


_NC_CACHE = {}


def kernel(**inputs):
    inp = {k: np.asarray(v) for k, v in inputs.items()}
    if "nc" not in _NC_CACHE:
        _NC_CACHE["nc"] = build(stage=99)
    nc = _NC_CACHE["nc"]
    maps = [prep_core(inp, c // 2, c % 2) for c in range(8)]
    res = run_bass_kernel_spmd(nc, maps, core_ids=list(range(8)))
    out = np.zeros((4, L, D), np.float32)
    for c in range(8):
        b, hf = c // 2, c % 2
        o = np.asarray(res.results[c]["out"], dtype=np.float32)
        if hf == 0:
            out[b, :OWN] = o
        else:
            out[b, OWN:] = o[::-1]
    return out
```
